# Optimizing a Trainium2 kernel written in Bass

```python
import jax, jax.numpy as jnp
from jax import lax
import numpy as np

D_MODEL = 1024
BATCH = 32
SEQ = 256
DEPTH = 4
DEC_BATCH = 8
DEC_SEQ = 1024
PAST_LEN = 256

GRID_W = 64
N_MIXERS = 2
N_RET_LAYERS = (DEPTH + 1) // 2
N_CONV_LAYERS = DEPTH // 2
RET_HEADS = 4
RET_DK = D_MODEL // RET_HEADS
RET_DV = 2 * D_MODEL // RET_HEADS
RET_CHUNK = 64
ROPE_BASE = 10000.0
CONV_WIDTH = 3
N_EXPERTS = 64
TOP_K = 6
N_GROUPS = 8
TOPK_GROUPS = 4
EXPERT_FF = 256
SHARED_FF = 256
ROUTED_SCALE = 2.5
MOE_BLOCK = 128
LN_EPS = 1e-5
DEEPNORM_ALPHA = (2.0 * DEPTH) ** 0.25
DEEPNORM_BETA = (8.0 * DEPTH) ** -0.25

kernel_name = 'hybrid_retention_shortconv_moe_dit_step'

F32 = jnp.float32


def layer_norm(x, g, b):
    xf = x.astype(F32)
    mu = xf.mean(-1, keepdims=True)
    var = jnp.square(xf - mu).mean(-1, keepdims=True)
    return ((xf - mu) * lax.rsqrt(var + LN_EPS) * g.astype(F32) + b.astype(F32)).astype(x.dtype)


def head_norm(o):
    mu = o.mean(-1, keepdims=True)
    var = jnp.square(o - mu).mean(-1, keepdims=True)
    return (o - mu) * lax.rsqrt(var + LN_EPS)


def rope_half(x, pos):
    half = x.shape[-1] // 2
    freqs = ROPE_BASE ** (-jnp.arange(half, dtype=F32) / half)
    ang = pos[:, None] * freqs[None, :]
    cos, sin = jnp.cos(ang), jnp.sin(ang)
    x1, x2 = x[..., :half], x[..., half:]
    return jnp.concatenate([x1 * cos - x2 * sin, x1 * sin + x2 * cos], axis=-1)


def rope_2d(x, row, col):
    h = x.shape[-1] // 2
    return jnp.concatenate([rope_half(x[..., :h], row), rope_half(x[..., h:], col)], axis=-1)


def grid_positions(rows):
    row = jnp.repeat(jnp.arange(rows, dtype=F32), GRID_W)
    col = jnp.tile(jnp.arange(GRID_W, dtype=F32), rows)
    return row, col


def retention_scan(q, k, v, log_gamma, s0):
    b, h, n, dk = q.shape
    dv = v.shape[-1]
    C = RET_CHUNK
    nc = n // C
    qc = q.reshape(b, h, nc, C, dk)
    kc = k.reshape(b, h, nc, C, dk)
    vc = v.reshape(b, h, nc, C, dv)
    pos = jnp.arange(C, dtype=F32)
    diff = pos[:, None] - pos[None, :]
    lg = log_gamma[:, None, None]
    intra_decay = jnp.where(diff >= 0, jnp.exp(lg * jnp.maximum(diff, 0.0)), 0.0)
    scores = jnp.einsum('bhncd,bhnmd->bhncm', qc, kc) * intra_decay[None, :, None]
    o_intra = jnp.einsum('bhncm,bhnme->bhnce', scores, vc)
    q_dec = jnp.exp(log_gamma[:, None] * (pos + 1.0))[None, :, None, :, None]
    k_dec = jnp.exp(log_gamma[:, None] * (C - 1.0 - pos))[None, :, None, :, None]
    chunk_dec = jnp.exp(log_gamma * C)[None, :, None, None]
    xs = (jnp.moveaxis(qc * q_dec, 2, 0), jnp.moveaxis(kc * k_dec, 2, 0), jnp.moveaxis(vc, 2, 0))

    def step(s, inp):
        qi, ki, vi = inp
        out = jnp.einsum('bhcd,bhde->bhce', qi, s)
        s = chunk_dec * s + jnp.einsum('bhcd,bhce->bhde', ki, vi)
        return s, out

    s_fin, o_inter = lax.scan(step, s0, xs)
    o = o_intra + jnp.moveaxis(o_inter, 0, 2)
    return o.reshape(b, h, n, dv), s_fin


def retention_mixer(h, w_in, w_out, decay_logit, s0_f, s0_b, pos):
    b, n, _ = h.shape
    proj = h @ w_in
    q, k, v, g = jnp.split(proj, [D_MODEL, 2 * D_MODEL, 4 * D_MODEL], axis=-1)

    def heads(t, d):
        return t.reshape(b, n, RET_HEADS, d).transpose(0, 2, 1, 3).astype(F32)

    q = heads(q, RET_DK)
    k = heads(k, RET_DK) * (RET_DK ** -0.5)
    v = heads(v, RET_DV)
    if pos is not None:
        row, col = pos
        q = rope_2d(q, row, col)
        k = rope_2d(k, row, col)
    log_gamma = jax.nn.log_sigmoid(decay_logit.astype(F32))
    o_f, s_f = retention_scan(q, k, v, log_gamma[0], s0_f)
    o_b, s_b = retention_scan(q[:, :, ::-1], k[:, :, ::-1], v[:, :, ::-1], log_gamma[1], s0_b)
    o = head_norm(o_f + o_b[:, :, ::-1])
    o = o.transpose(0, 2, 1, 3).reshape(b, n, RET_HEADS * RET_DV).astype(h.dtype)
    y = (jax.nn.silu(g) * o) @ w_out
    return y, s_f, s_b


def conv3(u, w):
    pad = [(0, 0)] * (u.ndim - 2) + [(1, 1), (0, 0)]
    up = jnp.pad(u, pad)
    return up[..., :-2, :] * w[0] + up[..., 1:-1, :] * w[1] + up[..., 2:, :] * w[2]


def short_conv_mixer(h, w_in, w_conv, w_out, rows):
    bg, cg, xt = jnp.split(h @ w_in, 3, axis=-1)
    u = cg * xt
    if rows is not None:
        b, n, d = u.shape
        cu = conv3(u.reshape(b, rows, GRID_W, d), w_conv).reshape(b, n, d)
    else:
        cu = conv3(u, w_conv)
    return (bg * cu) @ w_out


def moe_ffn(h, router_w, router_bias, w_gate, w_up, w_down, s_gate, s_up, s_down):
    b, n, d = h.shape
    t = h.reshape(b * n, d)
    scores = jax.nn.sigmoid((t @ router_w).astype(F32))
    sel = scores + router_bias.astype(F32)
    per_group = N_EXPERTS // N_GROUPS
    grp_score = lax.top_k(sel.reshape(-1, N_GROUPS, per_group), 2)[0].sum(-1)
    _, gidx = lax.top_k(grp_score, TOPK_GROUPS)
    gmask = jax.nn.one_hot(gidx, N_GROUPS, dtype=F32).sum(1) > 0
    emask = jnp.repeat(gmask, per_group, axis=-1)
    _, eidx = lax.top_k(jnp.where(emask, sel, -jnp.inf), TOP_K)
    wk = jnp.take_along_axis(scores, eidx, axis=-1)
    wk = wk / wk.sum(-1, keepdims=True) * ROUTED_SCALE
    combine = jnp.einsum('tk,tke->te', wk, jax.nn.one_hot(eidx, N_EXPERTS, dtype=F32)).astype(h.dtype)

    def expert_block(args):
        tb, cb = args
        hid = jax.nn.silu(jnp.einsum('td,edf->tef', tb, w_gate)) * jnp.einsum('td,edf->tef', tb, w_up)
        return jnp.einsum('tef,efd->td', hid * cb[:, :, None], w_down)

    nb = t.shape[0] // MOE_BLOCK
    routed = lax.map(expert_block, (t.reshape(nb, MOE_BLOCK, d),
                                    combine.reshape(nb, MOE_BLOCK, N_EXPERTS))).reshape(-1, d)
    shared = (jax.nn.silu(t @ s_gate) * (t @ s_up)) @ s_down
    return (routed + shared).reshape(b, n, d)


def run_trunk(x, cond, state_in, rows, ada_w, ada_b, ln_g, ln_b, ret_w_in, ret_w_out, ret_decay,
              conv_w_in, conv_w, conv_w_out, moe_router, moe_bias, moe_w_gate, moe_w_up, moe_w_down,
              shared_w_gate, shared_w_up, shared_w_down):
    b = x.shape[0]
    pos = None if rows is None else grid_positions(rows)
    new_states = []
    for i in range(DEPTH):
        j = i // N_MIXERS
        sh1, sc1, g1, sh2, sc2, g2 = jnp.split(jax.nn.silu(cond) @ ada_w[i] + ada_b[i], 6, axis=-1)
        h = x * (1 + sc1) + sh1
        if i % N_MIXERS == 0:
            if state_in is None:
                s0f = jnp.zeros((b, RET_HEADS, RET_DK, RET_DV), F32)
                s0b = s0f
            else:
                s0f = state_in[:, j, 0].astype(F32)
                s0b = state_in[:, j, 1].astype(F32)
            y, s_f, s_b = retention_mixer(h, ret_w_in[j], ret_w_out[j], ret_decay[j], s0f, s0b, pos)
            if state_in is None:
                new_states.append(jnp.stack([s_f, s_b], axis=1))
        else:
            y = short_conv_mixer(h, conv_w_in[j], conv_w[j], conv_w_out[j], rows)
        x = layer_norm(DEEPNORM_ALPHA * x + g1 * y, ln_g[i, 0], ln_b[i, 0])
        h = x * (1 + sc2) + sh2
        y = moe_ffn(h, moe_router[i], moe_bias[i], moe_w_gate[i], moe_w_up[i], moe_w_down[i],
                    shared_w_gate[i], shared_w_up[i], shared_w_down[i])
        x = layer_norm(DEEPNORM_ALPHA * x + g2 * y, ln_g[i, 1], ln_b[i, 1])
    return x, new_states


def setup_inputs(seed: int = 0) -> dict:
    key = jax.random.key(seed)
    ks = jax.random.split(key, 24)
    D = D_MODEL
    nrm = jax.random.normal
    gamma0 = 1.0 - 2.0 ** (-5.0 - np.arange(RET_HEADS, dtype=np.float32))
    logit0 = jnp.asarray(np.log(gamma0 / (1.0 - gamma0)), dtype=F32)
    return {
        'x_prompt': nrm(ks[0], (BATCH, SEQ, D), F32),
        'x_sample': nrm(ks[1], (DEC_BATCH, DEC_SEQ, D), F32),
        'state_ret': 0.5 * nrm(ks[2], (DEC_BATCH, N_RET_LAYERS, 2, RET_HEADS, RET_DK, RET_DV), F32),
        'c': nrm(ks[3], (DEC_BATCH, D), F32),
        'c_ctx': nrm(ks[4], (D,), F32),
        'ada_w': 0.5 * D ** -0.5 * nrm(ks[5], (DEPTH, D, 6 * D), F32),
        'ada_b': 0.02 * nrm(ks[6], (DEPTH, 6 * D), F32),
        'ln_g': 1.0 + 0.02 * nrm(ks[7], (DEPTH, 2, D), F32),
        'ln_b': 0.02 * nrm(ks[8], (DEPTH, 2, D), F32),
        'ret_w_in': D ** -0.5 * nrm(ks[9], (N_RET_LAYERS, D, 6 * D), F32),
        'ret_w_out': DEEPNORM_BETA * (2 * D) ** -0.5 * nrm(ks[10], (N_RET_LAYERS, 2 * D, D), F32),
        'ret_decay': logit0[None, None, :] + 0.1 * nrm(ks[11], (N_RET_LAYERS, 2, RET_HEADS), F32),
        'conv_w_in': D ** -0.5 * nrm(ks[12], (N_CONV_LAYERS, D, 3 * D), F32),
        'conv_w': CONV_WIDTH ** -0.5 * nrm(ks[13], (N_CONV_LAYERS, CONV_WIDTH, D), F32),
        'conv_w_out': DEEPNORM_BETA * D ** -0.5 * nrm(ks[14], (N_CONV_LAYERS, D, D), F32),
        'moe_router': D ** -0.5 * nrm(ks[15], (DEPTH, D, N_EXPERTS), F32),
        'moe_bias': 0.01 * nrm(ks[16], (DEPTH, N_EXPERTS), F32),
        'moe_w_gate': D ** -0.5 * nrm(ks[17], (DEPTH, N_EXPERTS, D, EXPERT_FF), F32),
        'moe_w_up': D ** -0.5 * nrm(ks[18], (DEPTH, N_EXPERTS, D, EXPERT_FF), F32),
        'moe_w_down': DEEPNORM_BETA * EXPERT_FF ** -0.5 * nrm(ks[19], (DEPTH, N_EXPERTS, EXPERT_FF, D), F32),
        'shared_w_gate': D ** -0.5 * nrm(ks[20], (DEPTH, D, SHARED_FF), F32),
        'shared_w_up': D ** -0.5 * nrm(ks[21], (DEPTH, D, SHARED_FF), F32),
        'shared_w_down': DEEPNORM_BETA * SHARED_FF ** -0.5 * nrm(ks[22], (DEPTH, SHARED_FF, D), F32),
    }


def reference(x_prompt, x_sample, state_ret, c, c_ctx, ada_w, ada_b, ln_g, ln_b, ret_w_in, ret_w_out,
              ret_decay, conv_w_in, conv_w, conv_w_out, moe_router, moe_bias, moe_w_gate, moe_w_up,
              moe_w_down, shared_w_gate, shared_w_up, shared_w_down):
    y_prompt, ctx_states = run_trunk(
        x_prompt, c_ctx[None, None, :], None, None, ada_w, ada_b, ln_g, ln_b, ret_w_in, ret_w_out,
        ret_decay, conv_w_in, conv_w, conv_w_out, moe_router, moe_bias, moe_w_gate, moe_w_up,
        moe_w_down, shared_w_gate, shared_w_up, shared_w_down)
    state_ret_new = jnp.stack(ctx_states, axis=1)
    rows = x_sample.shape[1] // GRID_W
    y_sample, _ = run_trunk(
        x_sample, c[:, None, :], state_ret, rows, ada_w, ada_b, ln_g, ln_b, ret_w_in, ret_w_out,
        ret_decay, conv_w_in, conv_w, conv_w_out, moe_router, moe_bias, moe_w_gate, moe_w_up,
        moe_w_down, shared_w_gate, shared_w_up, shared_w_down)
    return (y_prompt, y_sample, state_ret_new)
```

```python
import numpy as np
import concourse.bass as bass
import concourse.mybir as mybir
from concourse.bass_utils import run_bass_kernel_spmd

F32 = mybir.dt.float32
BF16 = mybir.dt.bfloat16
AF = mybir.ActivationFunctionType
ALU = mybir.AluOpType
AX = mybir.AxisListType

PE, ACT, DVE, POOL, SP = "tensor", "scalar", "vector", "gpsimd", "sync"
ALPHA = (2.0 * 4) ** 0.25
LN_EPS = 1e-5
T = 2048
NTT = 4


class Res:
    __slots__ = ("last_w", "readers", "arena", "psum")

    def __init__(self, arena=False, psum=False):
        self.last_w = None
        self.readers = []
        self.arena = arena
        self.psum = psum


class Op:
    __slots__ = ("eng", "fn", "deps", "signal", "semkey", "sigval", "is_dma")

    def __init__(self, eng, fn, is_dma, semkey):
        self.eng, self.fn, self.is_dma, self.semkey = eng, fn, is_dma, semkey
        self.deps, self.signal, self.sigval = (), False, None


class Prog:
    def __init__(self, nc):
        self.nc = nc
        self.ops = {e: [] for e in (PE, ACT, DVE, POOL, SP)}
        self.all_ops = []
        self.arena_tok = Res()

    def op(self, eng, fn, reads=(), writes=(), is_dma=False, semkey=None, pe_acc=False):
        o = Op(eng, fn, is_dma, semkey)
        reads = list(reads)
        if any(r.arena for r in reads) or any(w.arena for w in writes):
            reads.append(self.arena_tok)
        deps = set()
        for r in reads:
            if r.last_w is not None:
                deps.add(r.last_w)
            if r.psum:
                for rd in r.readers:
                    if rd.eng != eng:
                        deps.add(rd)
        for w in writes:
            if w.readers:
                deps.update(w.readers)
            elif w.last_w is not None:
                if not (pe_acc and eng == PE and w.last_w.eng == PE and not w.last_w.is_dma):
                    deps.add(w.last_w)
        for r in reads:
            r.readers.append(o)
        for w in writes:
            w.last_w = o
            w.readers = []
        deps.discard(o)
        o.deps = tuple(deps)
        self.ops[eng].append(o)
        self.all_ops.append(o)
        return o

    def emit(self, final_waits=()):
        nc = self.nc

        def qkey(o):
            return ("dma", o.semkey) if o.is_dma else o.eng

        order, cnt = {}, {}
        for o in self.all_ops:
            k = qkey(o)
            cnt[k] = cnt.get(k, 0) + 1
            order[o] = cnt[k]
        needed = {}
        for eng in self.ops:
            seen = {}
            for o in self.ops[eng]:
                best = {}
                for d in o.deps:
                    k = qkey(d)
                    od = order[d]
                    if od > seen.get(k, 0) and od > best.get(k, (0, None))[0]:
                        best[k] = (od, d)
                lst = []
                for k, (od, d) in best.items():
                    seen[k] = od
                    lst.append(d)
                    d.signal = True
                needed[o] = lst
        for d in final_waits:
            d.signal = True
        sems, sigcount = {}, {}
        for o in self.all_ops:
            if o.signal:
                k = qkey(o)
                if k not in sems:
                    sems[k] = nc.alloc_semaphore(name="s%d" % len(sems))
                sigcount[k] = sigcount.get(k, 0) + (16 if o.is_dma else 1)
                o.sigval = sigcount[k]
        self.n_sems = len(sems)
        self.sigcount = sigcount
        with nc.Block() as block:
            def mk(eng_name):
                def body(eng):
                    for o in self.ops[eng_name]:
                        for d in needed[o]:
                            eng.wait_ge(sems[qkey(d)], d.sigval)
                        ins = o.fn(eng)
                        if o.signal:
                            ins.then_inc(sems[qkey(o)], 16 if o.is_dma else 1)
                    if eng_name == SP:
                        for d in final_waits:
                            eng.wait_ge(sems[qkey(d)], d.sigval)
                return body
            block.sync(mk(SP))
            block.tensor(mk(PE))
            block.scalar(mk(ACT))
            block.vector(mk(DVE))
            block.gpsimd(mk(POOL))


def build(L=4, E=64, stop_after=None):
    NR, NCV = (L + 1) // 2, L // 2
    PG = E // 8
    nc = bass.Bass("TRN2", target_bir_lowering=False)
    P = Prog(nc)

    def din(name, shape, dt=F32):
        return nc.dram_tensor(name, list(shape), dt, kind="ExternalInput").ap()

    xT_d = din("xT", [1024, T])
    cond_d = din("condT", [128, 8, 2])
    state_d = din("state", [NR, 2, 4, 256, 512])
    adaw_d = din("ada_w", [L, 1024, 6144])
    adab_d = din("adab", [128, L, 48, 2])
    lnp_d = din("lnp", [128, L, 2, 2, 8])
    retin_d = din("ret_w_in", [NR, 1024, 6144])
    retout_d = din("ret_w_out", [NR, 2048, 1024])
    rdec_d = din("rdecay", [128, NR * 8])
    if NCV:
        cvin_d = din("conv_w_in", [NCV, 1024, 3072])
        cvw_d = din("convw", [128, NCV, 3, 8])
        cvout_d = din("conv_w_out", [NCV, 1024, 1024])
    rout_d = din("moe_router", [L, 1024, E])
    mbias_d = din("mbias", [128, L, E])
    wg_d = din("moe_w_gate", [L, E, 1024, 256])
    wu_d = din("moe_w_up", [L, E, 1024, 256])
    wd_d = din("moe_w_down", [L, E, 256, 1024])
    sg_d = din("shared_w_gate", [L, 1024, 256])
    su_d = din("shared_w_up", [L, 1024, 256])
    sd_d = din("shared_w_down", [L, 256, 1024])
    ident_d = din("ident", [128, 128])
    rot_d = din("rotm", [128, 128])
    delta_d = din("delta", [128, 1920])
    ip1_d = din("ip1", [128, 1024])
    jexp_d = din("jexp", [128, 4])
    rope_d = din("rope", [128, 160])
    yT_d = nc.dram_tensor("yT", [1024, T], F32, kind="ExternalOutput").ap()
    st_d = nc.dram_tensor("st", [4, NR, 2, 4, 256, 512], F32, kind="ExternalOutput").ap()

    SB_BYTES = 211840
    sb = nc.alloc_sbuf_tensor("sb", [128, SB_BYTES // 2], BF16)
    cur = [0]

    def view(off, dt, shape):
        n = int(np.prod(shape[1:])) * (4 if dt == F32 else 2)
        a = sb[:, off // 2:(off + n) // 2]
        if dt == F32:
            a = a.bitcast(F32)
        if len(shape) == 3:
            a = a.rearrange("p (a b) -> p a b", a=shape[1])
        elif len(shape) == 4:
            a = a.rearrange("p (a b c) -> p a b c", a=shape[1], b=shape[2])
        return a

    def alloc(dt, shape):
        n = int(np.prod(shape[1:])) * (4 if dt == F32 else 2)
        n = (n + 31) // 32 * 32
        off = cur[0]
        cur[0] += n
        return view(off, dt, shape)

    x = alloc(F32, [128, 8, T])
    hb = alloc(BF16, [128, 8, T])
    ws = [alloc(BF16, [128, 8, 512]) for _ in range(4)]
    wdS = [alloc(BF16, [128, 2, 1024]) for _ in range(2)]
    ident = alloc(F32, [128, 128])
    ones32 = alloc(F32, [128, 128])
    onesb = alloc(BF16, [128, 128])
    rot32 = None
    rotb = alloc(BF16, [128, 128])
    identb = alloc(BF16, [128, 128])
    mod = alloc(F32, [128, L, 48, 2])
    lnp = alloc(F32, [128, L, 2, 2, 8][0:1] + [L * 32])
    lnp = lnp.rearrange("p (l a b k) -> p l a b k", l=L, a=2, b=2)
    if NCV:
        cvw = alloc(F32, [128, NCV * 24]).rearrange("p (j c k) -> p j c k", j=NCV, c=3)
    mbias = alloc(F32, [128, L, E])
    lg = alloc(F32, [128, NR * 8])
    nlg = alloc(F32, [128, NR * 8])
    b1025 = alloc(F32, [128, NR * 8])
    lgtmp = alloc(F32, [128, NR * 8])
    decs = alloc(F32, [128, 4])
    jexp = alloc(F32, [128, 4])
    rope = alloc(F32, [128, 160])
    condT = alloc(F32, [128, 8, 2])
    scT = alloc(BF16, [128, 8, 2])
    ip1 = alloc(F32, [128, 1024])
    wr = alloc(BF16, [128, 8, E])
    ARENA0 = cur[0]
    ARENA = SB_BYTES - ARENA0
    assert ARENA >= 60928, ARENA

    def av(off, dt, shape):
        assert off + int(np.prod(shape[1:])) * (4 if dt == F32 else 2) <= ARENA, (off, shape)
        return view(ARENA0 + off, dt, shape)

    ps = [nc.alloc_psum_tensor("ps%d" % i, [128, 512], F32) for i in range(8)]
    rps = [Res(psum=True) for _ in range(8)]

    r_x = [[Res() for _ in range(NTT)] for _ in range(8)]
    r_hb = [[Res() for _ in range(NTT)] for _ in range(8)]
    r_wsA = [Res() for _ in range(4)]
    r_wsB = [Res() for _ in range(4)]
    r_wd = [Res() for _ in range(2)]
    r_c = Res()
    r_mod = Res()
    r_lg = Res()
    r_decs = Res()
    r_wr = Res()
    r_scT = Res()

    def mm(out, lhsT, rhs, start, stop, R, W, acc=None):
        P.op(PE, lambda e: e.matmul(out, lhsT=lhsT, rhs=rhs, start=start, stop=stop), reads=R, writes=[W],
             pe_acc=(not start) if acc is None else acc)

    def tr(out, in_, R, W):
        P.op(PE, lambda e: e.transpose(out, in_, ident), reads=R + [r_c], writes=[W], pe_acc=True)

    def act(out, in_, func, R, W, scale=1.0, bias=0.0):
        P.op(ACT, lambda e: e.activation(out=out, in_=in_, func=func, bias=bias, scale=scale), reads=R, writes=W)

    def tt(eng, out, in0, in1, op, R, W):
        P.op(eng, lambda e: e.tensor_tensor(out=out, in0=in0, in1=in1, op=op), reads=R, writes=W)

    def ts(eng, out, in0, s1, s2, op0, op1, R, W):
        if op1 is None:
            P.op(eng, lambda e: e.tensor_scalar(out=out, in0=in0, scalar1=s1, scalar2=None, op0=op0), reads=R, writes=W)
        else:
            P.op(eng, lambda e: e.tensor_scalar(out=out, in0=in0, scalar1=s1, scalar2=s2, op0=op0, op1=op1), reads=R, writes=W)

    def stt(eng, out, in0, scalar, in1, op0, op1, R, W):
        P.op(eng, lambda e: e.scalar_tensor_tensor(out=out, in0=in0, scalar=scalar, in1=in1, op0=op0, op1=op1), reads=R, writes=W)

    def cp(eng, out, in_, R, W):
        P.op(eng, lambda e: e.tensor_copy(out=out, in_=in_), reads=R, writes=W)

    dma_n = [0]

    def dma(eng, out, in_, R, W, key):
        return P.op(eng, lambda e: e.dma_start(out=out, in_=in_), reads=R, writes=W, is_dma=True, semkey=key)

    def wdma(slot, src, half=None):
        s = src.rearrange("(k p) f -> p k f", p=128)
        if half is None:
            dma(POOL, ws[slot], s, [], [r_wsA[slot], r_wsB[slot]], "ws%d" % slot)
        elif half == 0:
            dma(POOL, ws[slot][:, :, 0:256], s, [], [r_wsA[slot]], "wsA%d" % slot)
        else:
            dma(POOL, ws[slot][:, :, 256:512], s, [], [r_wsB[slot]], "wsB%d" % slot)

    def phase_switch():
        P.op(POOL, lambda e: e.memset(decs[:, 0:1], 0.0), reads=[], writes=[P.arena_tok, r_decs])

    def tsl(t_):
        return slice(t_ * 512, (t_ + 1) * 512)

    dma(SP, ident, ident_d, [], [r_c], "c0")
    dma(SP, ip1, ip1_d, [], [r_c], "c2")
    dma(SP, jexp, jexp_d, [], [r_c], "c3")
    dma(SP, rope, rope_d, [], [r_c], "c4")
    dma(SP, condT, cond_d, [], [r_c], "c5")
    dma(SP, mod, adab_d, [], [r_mod], "c6")
    dma(SP, lnp, lnp_d, [], [r_c], "c7")
    dma(SP, mbias, mbias_d, [], [r_c], "c8")
    dma(SP, lg, rdec_d, [], [r_lg], "c9")
    if NCV:
        dma(SP, cvw, cvw_d, [], [r_c], "c10")
    dma(POOL, rotb, rot_d, [], [r_c], "c11")
    dma(POOL, identb, ident_d, [], [r_c], "c12")
    for k in range(8):
        dma(SP, x[:, k, :], xT_d[k * 128:(k + 1) * 128, :], [], r_x[k], "x%d" % k)
    P.op(DVE, lambda e: e.memset(ones32, 1.0), writes=[r_c])
    P.op(DVE, lambda e: e.memset(onesb, 1.0), writes=[r_c])
    act(lgtmp, lg, AF.Exp, [r_lg], [r_lg], scale=-1.0)
    ts(DVE, lgtmp, lgtmp, 1.0, None, ALU.add, None, [r_lg], [r_lg])
    act(lgtmp, lgtmp, AF.Ln, [r_lg], [r_lg])
    ts(DVE, lg, lgtmp, -1.0, None, ALU.mult, None, [r_lg], [r_lg])
    cp(DVE, nlg, lgtmp, [r_lg], [r_lg])
    ts(DVE, b1025, lg, 1025.0, None, ALU.mult, None, [r_lg], [r_lg])
    act(scT, condT, AF.Silu, [r_c], [r_scT])
    for l in range(L):
        for blk in range(12):
            s = blk % 4
            wdma(s, adaw_d[l][:, blk * 512:(blk + 1) * 512])
            for j in range(4):
                oc = blk * 4 + j
                for k in range(8):
                    mm(ps[7][:, oc * 2:oc * 2 + 2], ws[s][:, k, j * 128:(j + 1) * 128], scT[:, k, :], k == 0, k == 7,
                       [r_wsA[s], r_wsB[s], r_scT], rps[7], acc=not (blk == 0 and j == 0 and k == 0))
        tt(DVE, mod[:, l].rearrange("p a b -> p (a b)"), mod[:, l].rearrange("p a b -> p (a b)"), ps[7][:, 0:96], ALU.add,
           [rps[7], r_mod], [r_mod])
        for base in (8, 32):
            f = 1.0 if (l == 0 and base == 8) else 1.0 / ALPHA
            v = mod[:, l, base:base + 8, :]
            ts(DVE, v, v, 1.0, f, ALU.add, ALU.mult, [r_mod], [r_mod])
    for l in range(L):
        for w in range(2):
            if not (l == L - 1 and w == 1):
                v = lnp[:, l, w]
                ts(DVE, v, v, ALPHA, None, ALU.mult, None, [r_c], [r_c])

    def mod_ap(l, which, k, cond):
        return mod[:, l, which * 8 + k, cond:cond + 1]

    def make_hb(l, which_sc, which_sh, eng_list):
        i = 0
        for k in range(8):
            for t_ in range(NTT):
                cond = 0 if t_ < 2 else 1
                ts(eng_list[i % len(eng_list)], hb[:, k, tsl(t_)], x[:, k, tsl(t_)], mod_ap(l, which_sc, k, cond), mod_ap(l, which_sh, k, cond),
                   ALU.mult, ALU.add, [r_x[k][t_], r_mod], [r_hb[k][t_]])
                i += 1

    def accum_x(psum_ap, l, which_g, dc, tok0, n):
        t_ = tok0 // 512
        cond = 0 if t_ < 2 else 1
        xs = x[:, dc, tok0:tok0 + n]
        stt(DVE, xs, psum_ap, mod_ap(l, which_g, dc, cond), xs, ALU.mult, ALU.add, None, None)

    def layer_norm(l, w, then_hb):
        phase_switch()
        S_ = [[av((i * 4 + t_) * 2048, F32, [128, 512]) for t_ in range(NTT)] for i in range(3)]
        r_S = [[Res(True) for _ in range(NTT)] for _ in range(3)]
        tmp = [av(24576 + i * 2048, F32, [128, 512]) for i in range(4)]
        r_tmp = [Res(True) for _ in range(4)]
        xsq = [av(32768 + i * 1024, BF16, [128, 512]) for i in range(4)]
        r_xsq = [Res(True) for _ in range(4)]
        n = 0
        for t_ in range(NTT):
            b1, b2 = 2 * t_, 2 * t_ + 1
            for k in range(8):
                q = n % 4
                n += 1
                act(xsq[q], x[:, k, tsl(t_)], AF.Square, [r_x[k][t_]], [r_xsq[q]])
                mm(ps[b2][:], onesb, xsq[q], k == 0, k == 7, [r_c, r_xsq[q]], rps[b2])
                mm(ps[b1][:], ones32, x[:, k, tsl(t_)], k == 0, k == 7, [r_c, r_x[k][t_]], rps[b1])
        for t_ in range(NTT):
            b1, b2 = 2 * t_, 2 * t_ + 1
            mean, tv, msq = S_[0][t_], S_[1][t_], S_[2][t_]
            ts(DVE, mean, ps[b1][:], 1.0 / 1024, None, ALU.mult, None, [rps[b1]], [r_S[0][t_]])
            tt(DVE, msq, mean, mean, ALU.mult, [r_S[0][t_]], [r_S[2][t_]])
            ts(DVE, tv, ps[b2][:], 1.0 / 1024, LN_EPS, ALU.mult, ALU.add, [rps[b2]], [r_S[1][t_]])
            tt(DVE, tv, tv, msq, ALU.subtract, [r_S[1][t_], r_S[2][t_]], [r_S[1][t_]])
        for t_ in range(NTT):
            act(S_[1][t_], S_[1][t_], AF.Sqrt, [r_S[1][t_]], [r_S[1][t_]])
        for t_ in range(NTT):
            P.op(DVE, lambda e, o=S_[1][t_]: e.reciprocal(out=o, in_=o), reads=[r_S[1][t_]], writes=[r_S[1][t_]])
        n = 0
        for t_ in range(NTT):
            cond = 0 if t_ < 2 else 1
            mean, rstd = S_[0][t_], S_[1][t_]
            for k in range(8):
                q = n % 4
                n += 1
                tt(DVE, tmp[q], x[:, k, tsl(t_)], mean, ALU.subtract, [r_x[k][t_], r_S[0][t_]], [r_tmp[q]])
                tt(DVE, tmp[q], tmp[q], rstd, ALU.mult, [r_tmp[q], r_S[1][t_]], [r_tmp[q]])
                act(x[:, k, tsl(t_)], tmp[q], AF.Identity, [r_tmp[q], r_c], [r_x[k][t_]], scale=lnp[:, l, w, 0, k:k + 1], bias=lnp[:, l, w, 1, k:k + 1])
                if then_hb:
                    ts(POOL, hb[:, k, tsl(t_)], x[:, k, tsl(t_)], mod_ap(l, 4, k, cond), mod_ap(l, 3, k, cond), ALU.mult, ALU.add,
                       [r_x[k][t_], r_mod], [r_hb[k][t_]])

    def retention(l, jl):
        phase_switch()
        qT = av(0, BF16, [128, 2, 1024]); r_qT = [[Res(True) for _ in range(2)] for _ in range(2)]
        kT = av(4096, BF16, [128, 2, 1024]); r_kT = [[Res(True) for _ in range(2)] for _ in range(2)]
        v = av(8192, BF16, [128, 8, 512]); r_v = [Res(True) for _ in range(8)]
        sg = av(16384, BF16, [128, 4, 1024]); r_sg = [[Res(True) for _ in range(2)] for _ in range(4)]
        S0 = av(24576, BF16, [128, 2, 2, 512]); r_S0 = [Res(True) for _ in range(2)]
        qdec = av(28672, BF16, [128, 2, 2, 512]); r_qdec = [Res(True) for _ in range(2)]
        kdec = av(24576, BF16, [128, 8, 2, 256]); r_kdec = r_S0 + r_qdec
        PT = [av(32768 + i * 1024, BF16, [128, 512]) for i in range(2)]; r_PT = [Res(True) for _ in range(2)]
        Ttab = av(34816, F32, [128, 1920]); r_T = Res(True)
        obf = av(42496, BF16, [128, 4, 512]); r_obf = Res(True)
        osq = av(46592, BF16, [128, 4, 512]); r_osq = Res(True)
        ttmp = av(42496, F32, [128, 1920])
        stt_ = [av(50688 + i * 2048, F32, [128, 512]) for i in range(3)]; r_st = [Res(True) for _ in range(3)]
        rowdec = av(56832, F32, [128, 512]); r_rowdec = Res(True)
        qraw = [av(58880 + i * 1024, BF16, [128, 512]) for i in range(2)]; r_qraw = [Res(True) for _ in range(2)]
        stage = [av(56832, F32, [128, 512]), av(58880, F32, [128, 512])]
        r_stage = [[r_rowdec], r_qraw]
        t_mean, t_rstd, t_1 = stt_
        pcnt = [0]

        def pbank():
            pcnt[0] += 1
            return 6 + pcnt[0] % 2

        for hd in range(4):
            li = jl * 8
            lgf = lg[:, li + hd:li + hd + 1]
            lgb = lg[:, li + 4 + hd:li + 4 + hd + 1]
            nlgb = nlg[:, li + 4 + hd:li + 4 + hd + 1]
            wdma(1, retin_d[jl][:, 2048 + hd * 512:2048 + (hd + 1) * 512])
            wdma(0, retin_d[jl][:, hd * 256:(hd + 1) * 256], half=0)
            wdma(0, retin_d[jl][:, 1024 + hd * 256:1024 + (hd + 1) * 256], half=1)
            wdma(2, retin_d[jl][:, 4096 + hd * 512:4096 + (hd + 1) * 512])
            dma(POOL, ws[3].rearrange("p k f -> p (k f)").rearrange("p (k f) -> p k f", k=4),
                retout_d[jl][hd * 512:(hd + 1) * 512, :].rearrange("(k p) f -> p k f", p=128), [], [r_wsA[3], r_wsB[3]], "ws3")
            wo = ws[3].rearrange("p k f -> p (k f)").rearrange("p (k f) -> p k f", k=4)
            def build_tables():
                dma(SP, Ttab, delta_d, [], [r_T], "dlt0")
                dma(SP, ttmp, delta_d, [], [r_obf, r_osq], "dlt1")
                ts(DVE, Ttab, Ttab, 0.0, None, ALU.max, None, [r_T], [r_T])
                act(Ttab, Ttab, AF.Exp, [r_T, r_lg], [r_T], scale=lgf)
                stt(DVE, Ttab, ttmp, 0.0, Ttab, ALU.is_equal, ALU.add, [r_obf, r_osq, r_T], [r_T])
                ts(DVE, ttmp, ttmp, 0.0, None, ALU.min, None, [r_obf, r_osq], [r_obf, r_osq])
                act(ttmp, ttmp, AF.Exp, [r_obf, r_osq, r_lg], [r_obf, r_osq], scale=nlgb)
                tt(DVE, Ttab, Ttab, ttmp, ALU.mult, [r_T, r_obf, r_osq], [r_T])
                act(decs[:, 0:2], jexp[:, 0:2], AF.Exp, [r_c, r_lg], [r_decs], scale=lgf)
                act(decs[:, 2:4], jexp[:, 2:4], AF.Exp, [r_c, r_lg], [r_decs], scale=lgb)
                ts(DVE, decs, decs, 0.0625, None, ALU.mult, None, [r_decs], [r_decs])
            for g in range(2):
                base = g * 1024
                cond = g
                isA = (g == 0)
                if isA:
                    for dr in range(2):
                        dma(POOL, S0[:, dr], state_d[jl, dr, hd].rearrange("(c p) e -> p c e", p=128), [], [r_S0[dr]], "S0%d" % dr)
                for tc in range(8):
                    b = pbank()
                    t_ = g * 2 + tc // 4
                    for k in range(8):
                        mm(ps[b][:], hb[:, k, base + tc * 128:base + (tc + 1) * 128], ws[1][:, k, :], k == 0, k == 7,
                           [r_wsA[1], r_wsB[1], r_hb[k][t_]], rps[b])
                    if tc % 2 == 0:
                        act(v[:, tc, :], ps[b][:], AF.Copy, [rps[b]], [r_v[tc]])
                    else:
                        cp(DVE, v[:, tc, :], ps[b][:], [rps[b]], [r_v[tc]])
                for c4 in range(4):
                    dest, rdest = (qT, r_qT) if c4 < 2 else (kT, r_kT)
                    c = c4 % 2
                    scl = 1.0 if c4 < 2 else 0.0625
                    rw = r_wsA[0] if c4 < 2 else r_wsB[0]
                    for t2 in range(2):
                        b = pbank()
                        t_ = g * 2 + t2
                        for k in range(8):
                            mm(ps[b][:], ws[0][:, k, c4 * 128:(c4 + 1) * 128], hb[:, k, tsl(t_)], k == 0, k == 7, [rw, r_hb[k][t_]], rps[b])
                        d_ap = dest[:, c, t2 * 512:(t2 + 1) * 512]
                        if not isA:
                            act(d_ap, ps[b][:], AF.Identity, [rps[b]], [rdest[c][t2]], scale=scl)
                        else:
                            qq = pcnt[0] % 2
                            act(qraw[qq], ps[b][:], AF.Identity, [rps[b]], [r_qraw[qq]], scale=scl)
                            b2 = pbank()
                            mm(ps[b2][:], rotb, qraw[qq], True, True, [r_c, r_qraw[qq]], rps[b2])
                            if c == 0:
                                cs = rope[:, 128 + t2 * 8:128 + (t2 + 1) * 8].unsqueeze(2).to_broadcast([128, 8, 64])
                                sn = rope[:, 144 + t2 * 8:144 + (t2 + 1) * 8].unsqueeze(2).to_broadcast([128, 8, 64])
                            else:
                                cs = rope[:, 0:64].unsqueeze(1).to_broadcast([128, 8, 64])
                                sn = rope[:, 64:128].unsqueeze(1).to_broadcast([128, 8, 64])
                            r3 = "p (r c) -> p r c"
                            tt(DVE, t_mean.rearrange(r3, r=8), qraw[qq].rearrange(r3, r=8), cs, ALU.mult, [r_qraw[qq], r_c], [r_st[0]])
                            tt(DVE, t_rstd.rearrange(r3, r=8), ps[b2][:].rearrange(r3, r=8), sn, ALU.mult, [rps[b2], r_c], [r_st[1]])
                            tt(DVE, d_ap, t_mean, t_rstd, ALU.add, [r_st[0], r_st[1]], [rdest[c][t2]])
                if isA:
                    build_tables()
                def deferred_proj():
                    for c in range(4):
                        for t2 in range(2):
                            b = pbank()
                            t_ = g * 2 + t2
                            for k in range(8):
                                mm(ps[b][:], ws[2][:, k, c * 128:(c + 1) * 128], hb[:, k, tsl(t_)], k == 0, k == 7,
                                   [r_wsA[2], r_wsB[2], r_hb[k][t_]], rps[b])
                            act(sg[:, c, t2 * 512:(t2 + 1) * 512], ps[b][:], AF.Silu, [rps[b]], [r_sg[c][t2]])
                    if not isA:
                        for tc in range(8):
                            b = pbank()
                            t_ = g * 2 + tc // 4
                            for k in range(8):
                                mm(ps[b][:, 0:256], hb[:, k, base + tc * 128:base + (tc + 1) * 128], ws[0][:, k, 256:512], k == 0, k == 7,
                                   [r_wsB[0], r_hb[k][t_]], rps[b])
                            hf = tc % 2
                            ts(DVE, kdec[:, tc, 0, :], ps[b][:, 0:256], decs[:, hf:hf + 1], None, ALU.mult, None, [rps[b], r_decs], r_kdec)
                            ts(DVE, kdec[:, tc, 1, :], ps[b][:, 0:256], decs[:, 2 + hf:3 + hf], None, ALU.mult, None, [rps[b], r_decs], r_kdec)
                        sc_ = 0
                        for s in range(4):
                            for dr in range(2):
                                for dc in range(2):
                                    b = pbank()
                                    for hf in range(2):
                                        tc = 2 * s + hf
                                        mm(ps[b][:], kdec[:, tc, dr, dc * 128:(dc + 1) * 128], v[:, tc, :], hf == 0, hf == 1,
                                           r_kdec + [r_v[tc]], rps[b])
                                    q = sc_ % 2
                                    sc_ += 1
                                    if q == 0:
                                        act(stage[q], ps[b][:], AF.Copy, [rps[b]], r_stage[q])
                                    else:
                                        cp(DVE, stage[q], ps[b][:], [rps[b]], r_stage[q])
                                    st_outs.append(dma(SP, st_d[s, jl, dr, hd, dc * 128:(dc + 1) * 128, :], stage[q], r_stage[q], [], "stg%d" % q))

                dstate = [True]
                seqs = [(0, 1024, 512)] if isA else [(s * 256, 256, 256) for s in range(4)]
                for (sb0, N, NI) in seqs:
                    NJ = N // 128
                    for it in range(N // NI):
                        i0 = it * NI
                        l0 = sb0 + i0
                        t2 = l0 // 512
                        if isA:
                            for dr in range(2):
                                if dr == 0:
                                    act(rowdec, ip1[:, i0:i0 + NI], AF.Exp, [r_c, r_lg], [r_rowdec], scale=lgf)
                                else:
                                    act(rowdec, ip1[:, i0:i0 + NI], AF.Exp, [r_c, r_lg], [r_rowdec], scale=nlgb,
                                        bias=b1025[:, li + 4 + hd:li + 4 + hd + 1])
                                for dc in range(2):
                                    tt(DVE, qdec[:, dr, dc, :], qT[:, dc, l0:l0 + NI], rowdec, ALU.mult, [r_qT[dc][t2], r_rowdec], [r_qdec[dr]])
                        for jc in range(NJ):
                            b = jc % 2
                            j0 = sb0 + jc * 128
                            for dc in range(2):
                                mm(ps[b][:, 0:NI], kT[:, dc, j0:j0 + 128], qT[:, dc, l0:l0 + NI], dc == 0, dc == 1,
                                   [r_kT[dc][j0 // 512], r_qT[dc][t2]], rps[b])
                            off = i0 - 128 * jc + 896
                            tt(DVE, PT[b][:, 0:NI], ps[b][:, 0:NI], Ttab[:, off:off + NI], ALU.mult, [rps[b], r_T], [r_PT[b]])
                            tcj = j0 // 128
                            for ec in range(4):
                                mm(ps[2 + ec][:, 0:NI], v[:, tcj, ec * 128:(ec + 1) * 128], PT[b][:, 0:NI], jc == 0, (jc == NJ - 1) and not isA,
                                   [r_v[tcj], r_PT[b]], rps[2 + ec])
                        if isA:
                            for dr in range(2):
                                for dc in range(2):
                                    for ec in range(4):
                                        mm(ps[2 + ec][:, 0:NI], S0[:, dr, dc, ec * 128:(ec + 1) * 128], qdec[:, dr, dc, :], False,
                                           dr == 1 and dc == 1, [r_S0[dr], r_qdec[dr]], rps[2 + ec])
                        if dstate[0]:
                            dstate[0] = False
                            deferred_proj()
                        for ec in range(4):
                            act(obf[:, ec, 0:NI], ps[2 + ec][:, 0:NI], AF.Copy, [rps[2 + ec]], [r_obf])
                            act(osq[:, ec, 0:NI], ps[2 + ec][:, 0:NI], AF.Square, [rps[2 + ec]], [r_osq])
                        for ec in range(4):
                            mm(ps[0][:, 0:NI], onesb, obf[:, ec, 0:NI], ec == 0, ec == 3, [r_c, r_obf], rps[0])
                        for ec in range(4):
                            mm(ps[1][:, 0:NI], onesb, osq[:, ec, 0:NI], ec == 0, ec == 3, [r_c, r_osq], rps[1])
                        m_, r_, t1 = t_mean[:, 0:NI], t_rstd[:, 0:NI], t_1[:, 0:NI]
                        ts(DVE, m_, ps[0][:, 0:NI], 1.0 / 512, None, ALU.mult, None, [rps[0]], [r_st[0]])
                        tt(DVE, t1, m_, m_, ALU.mult, [r_st[0]], [r_st[2]])
                        ts(DVE, r_, ps[1][:, 0:NI], 1.0 / 512, LN_EPS, ALU.mult, ALU.add, [rps[1]], [r_st[1]])
                        tt(DVE, r_, r_, t1, ALU.subtract, [r_st[1], r_st[2]], [r_st[1]])
                        act(r_, r_, AF.Sqrt, [r_st[1]], [r_st[1]])
                        P.op(DVE, lambda e, o=r_, i=r_: e.reciprocal(out=o, in_=i), reads=[r_st[1]], writes=[r_st[1]])
                        for ec in range(4):
                            tt(DVE, t1, ps[2 + ec][:, 0:NI], m_, ALU.subtract, [rps[2 + ec], r_st[0]], [r_st[2]])
                            tt(DVE, t1, t1, r_, ALU.mult, [r_st[2], r_st[1]], [r_st[2]])
                            sga = sg[:, ec, l0:l0 + NI]
                            tt(DVE, sga, t1, sga, ALU.mult, [r_st[2], r_sg[ec][t2]], [r_sg[ec][t2]])
                        for dc in range(8):
                            b = pbank()
                            for ec in range(4):
                                mm(ps[b][:, 0:NI], wo[:, ec, dc * 128:(dc + 1) * 128], sg[:, ec, l0:l0 + NI], ec == 0, ec == 3,
                                   [r_wsA[3], r_wsB[3], r_sg[ec][t2]], rps[b])
                            gt = base + l0
                            xs = x[:, dc, gt:gt + NI]
                            stt(DVE, xs, ps[b][:, 0:NI], mod_ap(l, 2, dc, cond), xs, ALU.mult, ALU.add,
                                [rps[b], r_mod, r_x[dc][gt // 512]], [r_x[dc][gt // 512]])

    def conv(l, jl):
        phase_switch()
        zT = av(0, BF16, [128, 8, T]); r_z = [[Res(True) for _ in range(NTT)] for _ in range(8)]
        cgs = [av(32768 + i * 2048, F32, [128, 512]) for i in range(2)]; r_cgs = [Res(True) for _ in range(2)]
        u = [av(36864 + i * 2048, F32, [128, 512]) for i in range(2)]; r_u = [Res(True) for _ in range(2)]
        cu = [av(40960 + i * 2048, F32, [128, 512]) for i in range(2)]; r_cu = [Res(True) for _ in range(2)]
        n = 0
        for half in range(2):
            for i in range(3):
                wdma(i, cvin_d[jl][:, i * 1024 + half * 512:i * 1024 + (half + 1) * 512])
            if half == 0:
                wdma(3, cvout_d[jl][:, 0:512])
            for c in range(4):
                cc = half * 4 + c
                for t_ in range(NTT):
                    q = n % 2
                    n += 1
                    bb = [q, 2 + q, 4 + q]
                    for i in range(3):
                        for k in range(8):
                            mm(ps[bb[i]][:], ws[i][:, k, c * 128:(c + 1) * 128], hb[:, k, tsl(t_)], k == 0, k == 7,
                               [r_wsA[i], r_wsB[i], r_hb[k][t_]], rps[bb[i]])
                    act(cgs[q], ps[bb[1]][:], AF.Copy, [rps[bb[1]]], [r_cgs[q]])
                    tt(DVE, u[q], cgs[q], ps[bb[2]][:], ALU.mult, [r_cgs[q], rps[bb[2]]], [r_u[q]])
                    act(cu[q], u[q], AF.Identity, [r_u[q], r_c], [r_cu[q]], scale=cvw[:, jl, 1, cc:cc + 1])
                    R_ = 8 if t_ < 2 else 2
                    u3 = u[q].rearrange("p (r c) -> p r c", r=R_)
                    c3 = cu[q].rearrange("p (r c) -> p r c", r=R_)
                    stt(DVE, c3[:, :, 1:], u3[:, :, :-1], cvw[:, jl, 0, cc:cc + 1], c3[:, :, 1:], ALU.mult, ALU.add, [r_u[q], r_cu[q], r_c], [r_cu[q]])
                    stt(DVE, c3[:, :, :-1], u3[:, :, 1:], cvw[:, jl, 2, cc:cc + 1], c3[:, :, :-1], ALU.mult, ALU.add, [r_u[q], r_cu[q], r_c], [r_cu[q]])
                    tt(DVE, zT[:, cc, tsl(t_)], ps[bb[0]][:], cu[q], ALU.mult, [rps[bb[0]], r_cu[q]], [r_z[cc][t_]])
        wdma(0, cvout_d[jl][:, 512:1024])
        n = 0
        for dcb in range(2):
            s = 3 if dcb == 0 else 0
            for j in range(4):
                dc = dcb * 4 + j
                for t_ in range(NTT):
                    b = 6 + n % 2
                    n += 1
                    for k in range(8):
                        mm(ps[b][:], ws[s][:, k, j * 128:(j + 1) * 128], zT[:, k, tsl(t_)], k == 0, k == 7,
                           [r_wsA[s], r_wsB[s], r_z[k][t_]], rps[b])
                    cond = 0 if t_ < 2 else 1
                    xs = x[:, dc, tsl(t_)]
                    stt(DVE, xs, ps[b][:], mod_ap(l, 2, dc, cond), xs, ALU.mult, ALU.add, [rps[b], r_mod, r_x[dc][t_]], [r_x[dc][t_]])

    def moe(l):
        phase_switch()
        hidb = [av(0, BF16, [128, 4, T]), av(32768, BF16, [128, 4, T])]
        r_hidb = [[[Res(True) for _ in range(NTT)] for _ in range(4)] for _ in range(2)]
        sgt = [av(16384 + i * 2048, F32, [128, 512]) for i in range(2)]; r_sgt = [Res(True) for _ in range(2)]
        tm = [av(20480 + i * 2048, F32, [128, 512]) for i in range(2)]; r_tm = [Res(True) for _ in range(2)]
        cHi = av(24576, BF16, [128, T]); cLo = av(28672, BF16, [128, T]); r_combT = [Res(True) for _ in range(NTT)]
        o = 32768
        SZ = 16 * E * 4
        scores = av(o, F32, [128, 16, E]); r_sc = Res(True)
        sel = av(o + SZ, F32, [128, 16, E]); r_sel = Res(True)
        tA = av(o + 2 * SZ, F32, [128, 16, E]); r_tA = Res(True)
        comb = av(o + 3 * SZ, F32, [128, 16, E]); r_comb = Res(True)
        o2 = o + 4 * SZ
        m1 = av(o2, F32, [128, 128]); m2 = av(o2 + 512, F32, [128, 128]); grp = av(o2 + 1024, F32, [128, 128])
        top8 = av(o2 + 1536, F32, [128, 16, 8]); gm = av(o2 + 2048, F32, [128, 128]); top8e = av(o2 + 2560, F32, [128, 16, 8])
        den = av(o2 + 3072, F32, [128, 16])
        r_sm = Res(True)

        dma(POOL, wr, rout_d[l].rearrange("(k p) f -> p k f", p=128), [], [r_wr], "wr")
        for tc in range(16):
            b = 6 + tc // 8
            t_ = tc // 4
            for k in range(8):
                mm(ps[b][:, (tc % 8) * E:(tc % 8 + 1) * E], hb[:, k, tc * 128:(tc + 1) * 128], wr[:, k, :], k == 0, k == 7,
                   [r_wr, r_hb[k][t_]], rps[b], acc=not (tc % 8 == 0 and k == 0))
        for b_ in range(2):
            act(scores[:, b_ * 8:(b_ + 1) * 8, :].rearrange("p a e -> p (a e)"), ps[6 + b_][:, 0:8 * E], AF.Sigmoid, [rps[6 + b_]], [r_sc])
        g3 = "p a (g e) -> p (a g) e"
        tt(DVE, sel, scores, mbias[:, l, :].unsqueeze(1).to_broadcast([128, 16, E]), ALU.add, [r_sc, r_c], [r_sel])
        selg = sel.rearrange(g3, g=8)
        tAg = tA.rearrange(g3, g=8)
        P.op(DVE, lambda e: e.tensor_reduce(out=m1, in_=selg, axis=AX.X, op=ALU.max), reads=[r_sel], writes=[r_sm])
        tt(DVE, tAg, selg, m1.unsqueeze(2).to_broadcast([128, 128, PG]), ALU.is_equal, [r_sel, r_sm], [r_tA])
        stt(DVE, tA, tA, -1e9, sel, ALU.mult, ALU.add, [r_tA, r_sel], [r_tA])
        P.op(DVE, lambda e: e.tensor_reduce(out=m2, in_=tAg, axis=AX.X, op=ALU.max), reads=[r_tA], writes=[r_sm])
        tt(DVE, grp, m1, m2, ALU.add, [r_sm], [r_sm])
        for tc in range(16):
            P.op(DVE, lambda e, tc=tc: e.max(out=top8[:, tc, :], in_=grp[:, tc * 8:(tc + 1) * 8]), reads=[r_sm], writes=[r_sm])
        tt(DVE, gm.rearrange("p (a g) -> p a g", a=16), grp.rearrange("p (a g) -> p a g", a=16),
           top8[:, :, 3:4].to_broadcast([128, 16, 8]), ALU.is_ge, [r_sm], [r_sm])
        ts(DVE, tA, sel, 2.0, None, ALU.add, None, [r_sel], [r_tA])
        tt(DVE, tAg, tAg, gm.unsqueeze(2).to_broadcast([128, 128, PG]), ALU.mult, [r_tA, r_sm], [r_tA])
        for tc in range(16):
            P.op(DVE, lambda e, tc=tc: e.max(out=top8e[:, tc, :], in_=tA[:, tc, :]), reads=[r_tA], writes=[r_sm])
        tt(DVE, sel, tA, top8e[:, :, 5:6].to_broadcast([128, 16, E]), ALU.is_ge, [r_tA, r_sm], [r_sel])
        tt(DVE, tA, scores, sel, ALU.mult, [r_sc, r_sel], [r_tA])
        P.op(DVE, lambda e: e.tensor_reduce(out=den, in_=tA, axis=AX.X, op=ALU.add), reads=[r_tA], writes=[r_sm])
        P.op(DVE, lambda e: e.reciprocal(out=den, in_=den), reads=[r_sm], writes=[r_sm])
        ts(DVE, den, den, 2.5, None, ALU.mult, None, [r_sm], [r_sm])
        tt(DVE, comb, tA, den.unsqueeze(2).to_broadcast([128, 16, E]), ALU.mult, [r_tA, r_sm], [r_comb])
        for tc in range(16):
            b = 6 + (tc // 4) % 2
            tr(ps[b][0:E, (tc % 4) * 128:(tc % 4 + 1) * 128], comb[:, tc, :], [r_comb], rps[b])
            if tc % 4 == 3:
                t_ = tc // 4
                act(cHi[0:E, tsl(t_)], ps[b][0:E, :], AF.Copy, [rps[b]], [r_combT[t_]])
                tt(DVE, cLo[0:E, tsl(t_)], ps[b][0:E, :], cHi[0:E, tsl(t_)], ALU.subtract, [rps[b], r_combT[t_]], [r_combT[t_]])

        NX = E + 1
        wdA = [av(52288, BF16, [128, 2, 1024]), av(52288 + 4096, BF16, [128, 2, 1024])]
        wd4 = [wdS[0], wdS[1], wdA[0], wdA[1]]
        r_wd4 = [r_wd[0], r_wd[1], Res(True), Res(True)]
        router_res = [r_sc, r_sel, r_tA, r_comb, r_sm]
        pairs = [[e for e in (2 * p, 2 * p + 1) if e < NX] for p in range((NX + 1) // 2)]

        def load_gu(e):
            s = e % 4
            if e < E:
                wdma(s, wg_d[l, e], half=0)
                wdma(s, wu_d[l, e], half=1)
            else:
                wdma(s, sg_d[l], half=0)
                wdma(s, su_d[l], half=1)

        def load_d(p):
            for e in pairs[p]:
                s = (p % 2) * 2 + e % 2
                src = wd_d[l, e] if e < E else sd_d[l]
                dma(POOL, wd4[s], src.rearrange("(k p) f -> p k f", p=128), [], [r_wd4[s]], "wd%d" % s)

        cnt = [0]
        dn = [0]
        first_b = [True] * 16

        def compute_block(p, e, t_):
            s = e % 4
            m = e % 2
            hid, r_hid = hidb[p % 2], r_hidb[p % 2]
            bcb = 4 if t_ % 2 == 0 else 6
            if e < E:
                sel1 = identb[0:E, e:e + 1].to_broadcast([E, 128])
                mm(ps[bcb][:], sel1, cHi[0:E, tsl(t_)], True, False, [r_c, r_combT[t_]], rps[bcb])
                mm(ps[bcb][:], sel1, cLo[0:E, tsl(t_)], False, True, [r_c, r_combT[t_]], rps[bcb])
            for fc in range(2):
                q = cnt[0] % 2
                cnt[0] += 1
                for k in range(8):
                    mm(ps[q][:], ws[s][:, k, fc * 128:(fc + 1) * 128], hb[:, k, tsl(t_)], k == 0, k == 7, [r_wsA[s], r_hb[k][t_]], rps[q])
                for k in range(8):
                    mm(ps[2 + q][:], ws[s][:, k, 256 + fc * 128:256 + (fc + 1) * 128], hb[:, k, tsl(t_)], k == 0, k == 7,
                       [r_wsB[s], r_hb[k][t_]], rps[2 + q])
                act(sgt[q], ps[q][:], AF.Silu, [rps[q]], [r_sgt[q]])
                hd_ = hid[:, m * 2 + fc, tsl(t_)]
                wres = [r_hid[m * 2 + fc][t_]]
                if p % 2 == 1 and first_b[(m * 2 + fc) * 4 + t_]:
                    first_b[(m * 2 + fc) * 4 + t_] = False
                    wres = wres + router_res
                if e < E:
                    tt(DVE, tm[q], sgt[q], ps[2 + q][:], ALU.mult, [r_sgt[q], rps[2 + q]], [r_tm[q]])
                    tt(DVE, hd_, tm[q], ps[bcb][:], ALU.mult, [r_tm[q], rps[bcb]], wres)
                else:
                    tt(DVE, hd_, sgt[q], ps[2 + q][:], ALU.mult, [r_sgt[q], rps[2 + q]], wres)

        def down_group(p, t_, dc):
            members = pairs[p]
            hid, r_hid = hidb[p % 2], r_hidb[p % 2]
            cond = 0 if t_ < 2 else 1
            b = 5 + dn[0] % 2 * 2
            dn[0] += 1
            n_ = len(members) * 2
            i = 0
            for e in members:
                m = e % 2
                s = (p % 2) * 2 + m
                for fc in range(2):
                    mm(ps[b][:], wd4[s][:, fc, dc * 128:(dc + 1) * 128], hid[:, m * 2 + fc, tsl(t_)], i == 0, i == n_ - 1,
                       [r_wd4[s], r_hid[m * 2 + fc][t_]], rps[b])
                    i += 1
            xs = x[:, dc, tsl(t_)]
            stt(DVE, xs, ps[b][:], mod_ap(l, 5, dc, cond), xs, ALU.mult, ALU.add, [rps[b], r_mod, r_x[dc][t_]], [r_x[dc][t_]])

        PF = 2
        for e in range(min(PF, NX)):
            load_gu(e)
        load_d(0)
        if len(pairs) > 1:
            load_d(1)
        pending = []
        for p, members in enumerate(pairs):
            nslots = len(members) * NTT
            per = -(-len(pending) // nslots) if pending else 0
            for e in members:
                if e + PF < NX:
                    load_gu(e + PF)
                for t_ in range(NTT):
                    compute_block(p, e, t_)
                    for _ in range(per):
                        if pending:
                            pp, tq, dq = pending.pop(0)
                            down_group(pp, tq, dq)
            while pending:
                pp, tq, dq = pending.pop(0)
                down_group(pp, tq, dq)
            if p >= 1 and p + 1 < len(pairs):
                load_d(p + 1)
            pending = [(p, tq, dq) for tq in range(NTT) for dq in range(8)]
        while pending:
            pp, tq, dq = pending.pop(0)
            down_group(pp, tq, dq)

    st_outs = []
    make_hb(0, 1, 0, [DVE, POOL])
    for k in range(8):
        for t_ in range(NTT):
            ts(POOL, x[:, k, tsl(t_)], x[:, k, tsl(t_)], ALPHA, None, ALU.mult, None, [r_x[k][t_]], [r_x[k][t_]])
    done = False
    for l in range(L):
        jl = l // 2
        if l > 0:
            make_hb(l, 1, 0, [DVE, POOL])
        if l % 2 == 0:
            retention(l, jl)
        else:
            conv(l, jl)
        if stop_after == (l, "mix"):
            break
        layer_norm(l, 0, True)
        if stop_after == (l, "ln1"):
            break
        moe(l)
        if stop_after == (l, "moe"):
            break
        layer_norm(l, 1, False)
    outs = []
    for k in range(8):
        outs.append(dma(SP, yT_d[k * 128:(k + 1) * 128, :], x[:, k, :], r_x[k], [], "out%d" % k))
    P.emit(final_waits=outs + st_outs[-2:])
    return nc, P


def const_inputs():
    p = np.arange(128)
    ident = np.eye(128, dtype=np.float32)
    rot = np.zeros((128, 128), np.float32)
    for m in range(64):
        rot[m + 64, m] = -1.0
        rot[m, m + 64] = 1.0
    delta = (np.arange(1920)[None, :] - p[:, None] - 896).astype(np.float32)
    ip1 = np.broadcast_to(np.arange(1, 1025, dtype=np.float32)[None, :], (128, 1024)).copy()
    jexp = np.stack([255.0 - p, 255.0 - (128 + p), 0.0 + p, 128.0 + p], axis=1).astype(np.float32)
    freqs = (10000.0 ** (-np.arange(64, dtype=np.float32) / 64)).astype(np.float32)
    fr = freqs[p % 64]
    angC = np.arange(64, dtype=np.float32)[None, :] * fr[:, None]
    angR = np.arange(16, dtype=np.float32)[None, :] * fr[:, None]
    rope = np.concatenate([np.cos(angC), np.sin(angC), np.cos(angR), np.sin(angR)], axis=1).astype(np.float32)
    return {"ident": ident, "rotm": rot, "delta": delta, "ip1": ip1, "jexp": jexp, "rope": rope}


def per_core_inputs(inp, core, L, E):
    NR, NCV = (L + 1) // 2, L // 2
    f = np.float32
    xs = np.asarray(inp["x_sample"][core], f)
    xp = np.asarray(inp["x_prompt"][4 * core:4 * core + 4], f).reshape(1024, 1024)
    xT = np.ascontiguousarray(np.concatenate([xs, xp], axis=0).T)
    cond = np.stack([np.asarray(inp["c"][core], f), np.asarray(inp["c_ctx"], f)], axis=1)
    condT = np.ascontiguousarray(cond.reshape(8, 128, 2).transpose(1, 0, 2))
    d = {"xT": xT, "condT": condT, "state": np.ascontiguousarray(np.asarray(inp["state_ret"][core], f))}
    return d


def shared_inputs(inp, L, E):
    NR, NCV = (L + 1) // 2, L // 2
    f = np.float32
    d = {}
    for k in ("ada_w", "ret_w_in", "ret_w_out", "moe_router", "moe_w_gate", "moe_w_up", "moe_w_down",
              "shared_w_gate", "shared_w_up", "shared_w_down"):
        d[k] = np.ascontiguousarray(np.asarray(inp[k], f))
    if NCV:
        d["conv_w_in"] = np.ascontiguousarray(np.asarray(inp["conv_w_in"], f))
        d["conv_w_out"] = np.ascontiguousarray(np.asarray(inp["conv_w_out"], f))
        cw = np.asarray(inp["conv_w"], f)
        d["convw"] = np.ascontiguousarray(cw.reshape(NCV, 3, 8, 128).transpose(3, 0, 1, 2))
    ab = np.asarray(inp["ada_b"], f).reshape(L, 48, 128).transpose(2, 0, 1)
    d["adab"] = np.ascontiguousarray(np.repeat(ab[:, :, :, None], 2, axis=3))
    g = np.asarray(inp["ln_g"], f).reshape(L, 2, 8, 128)
    b = np.asarray(inp["ln_b"], f).reshape(L, 2, 8, 128)
    d["lnp"] = np.ascontiguousarray(np.stack([g, b], axis=2).transpose(4, 0, 1, 2, 3))
    d["rdecay"] = np.ascontiguousarray(np.broadcast_to(np.asarray(inp["ret_decay"], f).reshape(1, NR * 8), (128, NR * 8)))
    d["mbias"] = np.ascontiguousarray(np.broadcast_to(np.asarray(inp["moe_bias"], f)[None], (128, L, E)))
    d.update(const_inputs())
    return d


_CACHE = {}


def run(inp, n_cores, L=4, E=64, stop_after=None, trace=False):
    key = (L, E, stop_after)
    if key not in _CACHE:
        _CACHE[key] = build(L, E, stop_after)
    nc, P = _CACHE[key]
    sh = shared_inputs(inp, L, E)
    in_maps = []
    for c in range(n_cores):
        m = dict(sh)
        m.update(per_core_inputs(inp, c, L, E))
        in_maps.append(m)
    res = run_bass_kernel_spmd(nc, in_maps, core_ids=list(range(n_cores)), trace=trace)
    return res


def kernel(**inputs):
    res = run(inputs, 8)
    NR = 2
    y_s = np.empty((8, 1024, 1024), np.float32)
    y_p = np.empty((32, 256, 1024), np.float32)
    st = np.empty((32, NR, 2, 4, 256, 512), np.float32)
    for c in range(8):
        yT = np.asarray(res.results[c]["yT"])
        y = yT.T
        y_s[c] = y[0:1024]
        y_p[4 * c:4 * c + 4] = y[1024:2048].reshape(4, 256, 1024)
        st[4 * c:4 * c + 4] = np.asarray(res.results[c]["st"])
    return (y_p, y_s, st)
```

```python
import numpy as np
import concourse.bass as bass
import concourse.mybir as mybir
from concourse.bass_utils import run_bass_kernel_spmd

F32 = mybir.dt.float32
BF16 = mybir.dt.bfloat16
AF = mybir.ActivationFunctionType
ALU = mybir.AluOpType
AX = mybir.AxisListType

PE, ACT, DVE, POOL, SP = "tensor", "scalar", "vector", "gpsimd", "sync"
ALPHA = (2.0 * 4) ** 0.25
LN_EPS = 1e-5
T = 2048
NTT = 4


class Res:
    __slots__ = ("last_w", "readers", "arena", "psum")

    def __init__(self, arena=False, psum=False):
        self.last_w = None
        self.readers = []
        self.arena = arena
        self.psum = psum


class Op:
    __slots__ = ("eng", "fn", "deps", "signal", "semkey", "sigval", "is_dma")

    def __init__(self, eng, fn, is_dma, semkey):
        self.eng, self.fn, self.is_dma, self.semkey = eng, fn, is_dma, semkey
        self.deps, self.signal, self.sigval = (), False, None


class Prog:
    def __init__(self, nc):
        self.nc = nc
        self.ops = {e: [] for e in (PE, ACT, DVE, POOL, SP)}
        self.all_ops = []
        self.arena_tok = Res()

    def op(self, eng, fn, reads=(), writes=(), is_dma=False, semkey=None, pe_acc=False):
        o = Op(eng, fn, is_dma, semkey)
        reads = list(reads)
        if any(r.arena for r in reads) or any(w.arena for w in writes):
            reads.append(self.arena_tok)
        deps = set()
        for r in reads:
            if r.last_w is not None:
                deps.add(r.last_w)
            if r.psum:
                for rd in r.readers:
                    if rd.eng != eng:
                        deps.add(rd)
        for w in writes:
            if w.readers:
                deps.update(w.readers)
            elif w.last_w is not None:
                if not (pe_acc and eng == PE and w.last_w.eng == PE and not w.last_w.is_dma):
                    deps.add(w.last_w)
        for r in reads:
            r.readers.append(o)
        for w in writes:
            w.last_w = o
            w.readers = []
        deps.discard(o)
        o.deps = tuple(deps)
        self.ops[eng].append(o)
        self.all_ops.append(o)
        return o

    def emit(self, final_waits=()):
        nc = self.nc

        def qkey(o):
            return ("dma", o.semkey) if o.is_dma else o.eng

        order, cnt = {}, {}
        for o in self.all_ops:
            k = qkey(o)
            cnt[k] = cnt.get(k, 0) + 1
            order[o] = cnt[k]
        needed = {}
        for eng in self.ops:
            seen = {}
            for o in self.ops[eng]:
                best = {}
                for d in o.deps:
                    k = qkey(d)
                    od = order[d]
                    if od > seen.get(k, 0) and od > best.get(k, (0, None))[0]:
                        best[k] = (od, d)
                lst = []
                for k, (od, d) in best.items():
                    seen[k] = od
                    lst.append(d)
                    d.signal = True
                needed[o] = lst
        for d in final_waits:
            d.signal = True
        sems, sigcount = {}, {}
        for o in self.all_ops:
            if o.signal:
                k = qkey(o)
                if k not in sems:
                    sems[k] = nc.alloc_semaphore(name="s%d" % len(sems))
                sigcount[k] = sigcount.get(k, 0) + (16 if o.is_dma else 1)
                o.sigval = sigcount[k]
        self.n_sems = len(sems)
        self.sigcount = sigcount
        with nc.Block() as block:
            def mk(eng_name):
                def body(eng):
                    for o in self.ops[eng_name]:
                        for d in needed[o]:
                            eng.wait_ge(sems[qkey(d)], d.sigval)
                        ins = o.fn(eng)
                        if o.signal:
                            ins.then_inc(sems[qkey(o)], 16 if o.is_dma else 1)
                    if eng_name == SP:
                        for d in final_waits:
                            eng.wait_ge(sems[qkey(d)], d.sigval)
                return body
            block.sync(mk(SP))
            block.tensor(mk(PE))
            block.scalar(mk(ACT))
            block.vector(mk(DVE))
            block.gpsimd(mk(POOL))


def build(L=4, E=64, stop_after=None):
    NR, NCV = (L + 1) // 2, L // 2
    PG = E // 8
    nc = bass.Bass("TRN2", target_bir_lowering=False)
    P = Prog(nc)

    def din(name, shape, dt=F32):
        return nc.dram_tensor(name, list(shape), dt, kind="ExternalInput").ap()

    xT_d = din("xT", [1024, T])
    cond_d = din("condT", [128, 8, 2])
    state_d = din("state", [NR, 2, 4, 256, 512])
    adaw_d = din("ada_w", [L, 1024, 6144])
    adab_d = din("adab", [128, L, 48, 2])
    lnp_d = din("lnp", [128, L, 2, 2, 8])
    retin_d = din("ret_w_in", [NR, 1024, 6144])
    retout_d = din("ret_w_out", [NR, 2048, 1024])
    rdec_d = din("rdecay", [128, NR * 8])
    if NCV:
        cvin_d = din("conv_w_in", [NCV, 1024, 3072])
        cvw_d = din("convw", [128, NCV, 3, 8])
        cvout_d = din("conv_w_out", [NCV, 1024, 1024])
    rout_d = din("moe_router", [L, 1024, E])
    mbias_d = din("mbias", [128, L, E])
    wg_d = din("moe_w_gate", [L, E, 1024, 256])
    wu_d = din("moe_w_up", [L, E, 1024, 256])
    wd_d = din("moe_w_down", [L, E, 256, 1024])
    sg_d = din("shared_w_gate", [L, 1024, 256])
    su_d = din("shared_w_up", [L, 1024, 256])
    sd_d = din("shared_w_down", [L, 256, 1024])
    ident_d = din("ident", [128, 128])
    rot_d = din("rotm", [128, 128])
    delta_d = din("delta", [128, 1920])
    ip1_d = din("ip1", [128, 1024])
    jexp_d = din("jexp", [128, 4])
    rope_d = din("rope", [128, 160])
    yT_d = nc.dram_tensor("yT", [1024, T], F32, kind="ExternalOutput").ap()
    st_d = nc.dram_tensor("st", [4, NR, 2, 4, 256, 512], F32, kind="ExternalOutput").ap()

    SB_BYTES = 211840
    sb = nc.alloc_sbuf_tensor("sb", [128, SB_BYTES // 2], BF16)
    cur = [0]

    def view(off, dt, shape):
        n = int(np.prod(shape[1:])) * (4 if dt == F32 else 2)
        a = sb[:, off // 2:(off + n) // 2]
        if dt == F32:
            a = a.bitcast(F32)
        if len(shape) == 3:
            a = a.rearrange("p (a b) -> p a b", a=shape[1])
        elif len(shape) == 4:
            a = a.rearrange("p (a b c) -> p a b c", a=shape[1], b=shape[2])
        return a

    def alloc(dt, shape):
        n = int(np.prod(shape[1:])) * (4 if dt == F32 else 2)
        n = (n + 31) // 32 * 32
        off = cur[0]
        cur[0] += n
        return view(off, dt, shape)

    x = alloc(F32, [128, 8, T])
    hb = alloc(BF16, [128, 8, T])
    ws = [alloc(BF16, [128, 8, 512]) for _ in range(4)]
    wdS = [alloc(BF16, [128, 2, 1024]) for _ in range(2)]
    ident = alloc(F32, [128, 128])
    ones32 = alloc(F32, [128, 128])
    onesb = alloc(BF16, [128, 128])
    rot32 = None
    rotb = alloc(BF16, [128, 128])
    identb = alloc(BF16, [128, 128])
    mod = alloc(F32, [128, L, 48, 2])
    lnp = alloc(F32, [128, L, 2, 2, 8][0:1] + [L * 32])
    lnp = lnp.rearrange("p (l a b k) -> p l a b k", l=L, a=2, b=2)
    if NCV:
        cvw = alloc(F32, [128, NCV * 24]).rearrange("p (j c k) -> p j c k", j=NCV, c=3)
    mbias = alloc(F32, [128, L, E])
    lg = alloc(F32, [128, NR * 8])
    nlg = alloc(F32, [128, NR * 8])
    b1025 = alloc(F32, [128, NR * 8])
    lgtmp = alloc(F32, [128, NR * 8])
    decs = alloc(F32, [128, 4])
    jexp = alloc(F32, [128, 4])
    rope = alloc(F32, [128, 160])
    condT = alloc(F32, [128, 8, 2])
    scT = alloc(BF16, [128, 8, 2])
    ip1 = alloc(F32, [128, 1024])
    wr = alloc(BF16, [128, 8, E])
    ARENA0 = cur[0]
    ARENA = SB_BYTES - ARENA0
    assert ARENA >= 60928, ARENA

    def av(off, dt, shape):
        assert off + int(np.prod(shape[1:])) * (4 if dt == F32 else 2) <= ARENA, (off, shape)
        return view(ARENA0 + off, dt, shape)

    ps = [nc.alloc_psum_tensor("ps%d" % i, [128, 512], F32) for i in range(8)]
    rps = [Res(psum=True) for _ in range(8)]

    r_x = [[Res() for _ in range(NTT)] for _ in range(8)]
    r_hb = [[Res() for _ in range(NTT)] for _ in range(8)]
    r_wsA = [Res() for _ in range(4)]
    r_wsB = [Res() for _ in range(4)]
    r_wd = [Res() for _ in range(2)]
    r_c = Res()
    r_mod = Res()
    r_lg = Res()
    r_decs = Res()
    r_wr = Res()
    r_scT = Res()

    def mm(out, lhsT, rhs, start, stop, R, W, acc=None):
        P.op(PE, lambda e: e.matmul(out, lhsT=lhsT, rhs=rhs, start=start, stop=stop), reads=R, writes=[W],
             pe_acc=(not start) if acc is None else acc)

    def tr(out, in_, R, W):
        P.op(PE, lambda e: e.transpose(out, in_, ident), reads=R + [r_c], writes=[W], pe_acc=True)

    def act(out, in_, func, R, W, scale=1.0, bias=0.0):
        P.op(ACT, lambda e: e.activation(out=out, in_=in_, func=func, bias=bias, scale=scale), reads=R, writes=W)

    def tt(eng, out, in0, in1, op, R, W):
        P.op(eng, lambda e: e.tensor_tensor(out=out, in0=in0, in1=in1, op=op), reads=R, writes=W)

    def ts(eng, out, in0, s1, s2, op0, op1, R, W):
        if op1 is None:
            P.op(eng, lambda e: e.tensor_scalar(out=out, in0=in0, scalar1=s1, scalar2=None, op0=op0), reads=R, writes=W)
        else:
            P.op(eng, lambda e: e.tensor_scalar(out=out, in0=in0, scalar1=s1, scalar2=s2, op0=op0, op1=op1), reads=R, writes=W)

    def stt(eng, out, in0, scalar, in1, op0, op1, R, W):
        P.op(eng, lambda e: e.scalar_tensor_tensor(out=out, in0=in0, scalar=scalar, in1=in1, op0=op0, op1=op1), reads=R, writes=W)

    def cp(eng, out, in_, R, W):
        P.op(eng, lambda e: e.tensor_copy(out=out, in_=in_), reads=R, writes=W)

    dma_n = [0]

    def dma(eng, out, in_, R, W, key):
        return P.op(eng, lambda e: e.dma_start(out=out, in_=in_), reads=R, writes=W, is_dma=True, semkey=key)

    def wdma(slot, src, half=None):
        s = src.rearrange("(k p) f -> p k f", p=128)
        if half is None:
            dma(POOL, ws[slot], s, [], [r_wsA[slot], r_wsB[slot]], "ws%d" % slot)
        elif half == 0:
            dma(POOL, ws[slot][:, :, 0:256], s, [], [r_wsA[slot]], "wsA%d" % slot)
        else:
            dma(POOL, ws[slot][:, :, 256:512], s, [], [r_wsB[slot]], "wsB%d" % slot)

    def phase_switch():
        P.op(POOL, lambda e: e.memset(decs[:, 0:1], 0.0), reads=[], writes=[P.arena_tok, r_decs])

    def tsl(t_):
        return slice(t_ * 512, (t_ + 1) * 512)

    dma(SP, ident, ident_d, [], [r_c], "c0")
    dma(SP, ip1, ip1_d, [], [r_c], "c2")
    dma(SP, jexp, jexp_d, [], [r_c], "c3")
    dma(SP, rope, rope_d, [], [r_c], "c4")
    dma(SP, condT, cond_d, [], [r_c], "c5")
    dma(SP, mod, adab_d, [], [r_mod], "c6")
    dma(SP, lnp, lnp_d, [], [r_c], "c7")
    dma(SP, mbias, mbias_d, [], [r_c], "c8")
    dma(SP, lg, rdec_d, [], [r_lg], "c9")
    if NCV:
        dma(SP, cvw, cvw_d, [], [r_c], "c10")
    dma(POOL, rotb, rot_d, [], [r_c], "c11")
    dma(POOL, identb, ident_d, [], [r_c], "c12")
    for k in range(8):
        dma(SP, x[:, k, :], xT_d[k * 128:(k + 1) * 128, :], [], r_x[k], "x%d" % k)
    P.op(DVE, lambda e: e.memset(ones32, 1.0), writes=[r_c])
    P.op(DVE, lambda e: e.memset(onesb, 1.0), writes=[r_c])
    act(lgtmp, lg, AF.Exp, [r_lg], [r_lg], scale=-1.0)
    ts(DVE, lgtmp, lgtmp, 1.0, None, ALU.add, None, [r_lg], [r_lg])
    act(lgtmp, lgtmp, AF.Ln, [r_lg], [r_lg])
    ts(DVE, lg, lgtmp, -1.0, None, ALU.mult, None, [r_lg], [r_lg])
    cp(DVE, nlg, lgtmp, [r_lg], [r_lg])
    ts(DVE, b1025, lg, 1025.0, None, ALU.mult, None, [r_lg], [r_lg])
    act(scT, condT, AF.Silu, [r_c], [r_scT])
    for l in range(L):
        for blk in range(12):
            s = blk % 4
            wdma(s, adaw_d[l][:, blk * 512:(blk + 1) * 512])
            for j in range(4):
                oc = blk * 4 + j
                for k in range(8):
                    mm(ps[7][:, oc * 2:oc * 2 + 2], ws[s][:, k, j * 128:(j + 1) * 128], scT[:, k, :], k == 0, k == 7,
                       [r_wsA[s], r_wsB[s], r_scT], rps[7], acc=not (blk == 0 and j == 0 and k == 0))
        tt(DVE, mod[:, l].rearrange("p a b -> p (a b)"), mod[:, l].rearrange("p a b -> p (a b)"), ps[7][:, 0:96], ALU.add,
           [rps[7], r_mod], [r_mod])
        for base in (8, 32):
            f = 1.0 if (l == 0 and base == 8) else 1.0 / ALPHA
            v = mod[:, l, base:base + 8, :]
            ts(DVE, v, v, 1.0, f, ALU.add, ALU.mult, [r_mod], [r_mod])
    for l in range(L):
        for w in range(2):
            if not (l == L - 1 and w == 1):
                v = lnp[:, l, w]
                ts(DVE, v, v, ALPHA, None, ALU.mult, None, [r_c], [r_c])

    def mod_ap(l, which, k, cond):
        return mod[:, l, which * 8 + k, cond:cond + 1]

    def make_hb(l, which_sc, which_sh, eng_list):
        i = 0
        for k in range(8):
            for t_ in range(NTT):
                cond = 0 if t_ < 2 else 1
                ts(eng_list[i % len(eng_list)], hb[:, k, tsl(t_)], x[:, k, tsl(t_)], mod_ap(l, which_sc, k, cond), mod_ap(l, which_sh, k, cond),
                   ALU.mult, ALU.add, [r_x[k][t_], r_mod], [r_hb[k][t_]])
                i += 1

    def accum_x(psum_ap, l, which_g, dc, tok0, n):
        t_ = tok0 // 512
        cond = 0 if t_ < 2 else 1
        xs = x[:, dc, tok0:tok0 + n]
        stt(DVE, xs, psum_ap, mod_ap(l, which_g, dc, cond), xs, ALU.mult, ALU.add, None, None)

    def layer_norm(l, w, then_hb):
        phase_switch()
        S_ = [[av((i * 4 + t_) * 2048, F32, [128, 512]) for t_ in range(NTT)] for i in range(3)]
        r_S = [[Res(True) for _ in range(NTT)] for _ in range(3)]
        tmp = [av(24576 + i * 2048, F32, [128, 512]) for i in range(4)]
        r_tmp = [Res(True) for _ in range(4)]
        xsq = [av(32768 + i * 1024, BF16, [128, 512]) for i in range(4)]
        r_xsq = [Res(True) for _ in range(4)]
        xbf = [av(36864 + i * 1024, BF16, [128, 512]) for i in range(4)]
        r_xbf = [Res(True) for _ in range(4)]
        n = 0
        for t_ in range(NTT):
            b1, b2 = 2 * t_, 2 * t_ + 1
            for k in range(8):
                q = n % 4
                n += 1
                act(xsq[q], x[:, k, tsl(t_)], AF.Square, [r_x[k][t_]], [r_xsq[q]])
                mm(ps[b2][:], onesb, xsq[q], k == 0, k == 7, [r_c, r_xsq[q]], rps[b2])
                cp(DVE, xbf[q], x[:, k, tsl(t_)], [r_x[k][t_]], [r_xbf[q]])
                mm(ps[b1][:], onesb, xbf[q], k == 0, k == 7, [r_c, r_xbf[q]], rps[b1])
        for t_ in range(NTT):
            b1, b2 = 2 * t_, 2 * t_ + 1
            mean, tv, msq = S_[0][t_], S_[1][t_], S_[2][t_]
            ts(DVE, mean, ps[b1][:], 1.0 / 1024, None, ALU.mult, None, [rps[b1]], [r_S[0][t_]])
            tt(DVE, msq, mean, mean, ALU.mult, [r_S[0][t_]], [r_S[2][t_]])
            ts(DVE, tv, ps[b2][:], 1.0 / 1024, LN_EPS, ALU.mult, ALU.add, [rps[b2]], [r_S[1][t_]])
            tt(DVE, tv, tv, msq, ALU.subtract, [r_S[1][t_], r_S[2][t_]], [r_S[1][t_]])
        for t_ in range(NTT):
            act(S_[1][t_], S_[1][t_], AF.Sqrt, [r_S[1][t_]], [r_S[1][t_]])
        for t_ in range(NTT):
            P.op(DVE, lambda e, o=S_[1][t_]: e.reciprocal(out=o, in_=o), reads=[r_S[1][t_]], writes=[r_S[1][t_]])
        n = 0
        for t_ in range(NTT):
            cond = 0 if t_ < 2 else 1
            mean, rstd = S_[0][t_], S_[1][t_]
            for k in range(8):
                q = n % 4
                n += 1
                tt(DVE, tmp[q], x[:, k, tsl(t_)], mean, ALU.subtract, [r_x[k][t_], r_S[0][t_]], [r_tmp[q]])
                tt(DVE, tmp[q], tmp[q], rstd, ALU.mult, [r_tmp[q], r_S[1][t_]], [r_tmp[q]])
                act(x[:, k, tsl(t_)], tmp[q], AF.Identity, [r_tmp[q], r_c], [r_x[k][t_]], scale=lnp[:, l, w, 0, k:k + 1], bias=lnp[:, l, w, 1, k:k + 1])
                if then_hb:
                    ts(POOL, hb[:, k, tsl(t_)], x[:, k, tsl(t_)], mod_ap(l, 4, k, cond), mod_ap(l, 3, k, cond), ALU.mult, ALU.add,
                       [r_x[k][t_], r_mod], [r_hb[k][t_]])

    def retention(l, jl):
        phase_switch()
        qT = av(0, BF16, [128, 2, 1024]); r_qT = [[Res(True) for _ in range(2)] for _ in range(2)]
        kT = av(4096, BF16, [128, 2, 1024]); r_kT = [[Res(True) for _ in range(2)] for _ in range(2)]
        v = av(8192, BF16, [128, 8, 512]); r_v = [Res(True) for _ in range(8)]
        sg = av(16384, BF16, [128, 4, 1024]); r_sg = [[Res(True) for _ in range(2)] for _ in range(4)]
        S0 = av(24576, BF16, [128, 2, 2, 512]); r_S0 = [Res(True) for _ in range(2)]
        qdec = av(28672, BF16, [128, 2, 2, 512]); r_qdec = [Res(True) for _ in range(2)]
        kdec = av(24576, BF16, [128, 8, 2, 256]); r_kdec = r_S0 + r_qdec
        PT = [av(32768 + i * 1024, BF16, [128, 512]) for i in range(2)]; r_PT = [Res(True) for _ in range(2)]
        Ttab = av(34816, F32, [128, 1920]); r_T = Res(True)
        obf = av(42496, BF16, [128, 4, 512]); r_obf = Res(True)
        osq = av(46592, BF16, [128, 4, 512]); r_osq = Res(True)
        ttmp = av(42496, F32, [128, 1920])
        stt_ = [av(50688 + i * 2048, F32, [128, 512]) for i in range(3)]; r_st = [Res(True) for _ in range(3)]
        rowdec = av(56832, F32, [128, 512]); r_rowdec = Res(True)
        qraw = [av(58880 + i * 1024, BF16, [128, 512]) for i in range(2)]; r_qraw = [Res(True) for _ in range(2)]
        stage = [av(56832, F32, [128, 512]), av(58880, F32, [128, 512])]
        r_stage = [[r_rowdec], r_qraw]
        t_mean, t_rstd, t_1 = stt_
        pcnt = [0]

        def pbank():
            pcnt[0] += 1
            return 6 + pcnt[0] % 2

        for hd in range(4):
            li = jl * 8
            lgf = lg[:, li + hd:li + hd + 1]
            lgb = lg[:, li + 4 + hd:li + 4 + hd + 1]
            nlgb = nlg[:, li + 4 + hd:li + 4 + hd + 1]
            wdma(1, retin_d[jl][:, 2048 + hd * 512:2048 + (hd + 1) * 512])
            wdma(0, retin_d[jl][:, hd * 256:(hd + 1) * 256], half=0)
            wdma(0, retin_d[jl][:, 1024 + hd * 256:1024 + (hd + 1) * 256], half=1)
            wdma(2, retin_d[jl][:, 4096 + hd * 512:4096 + (hd + 1) * 512])
            dma(POOL, ws[3].rearrange("p k f -> p (k f)").rearrange("p (k f) -> p k f", k=4),
                retout_d[jl][hd * 512:(hd + 1) * 512, :].rearrange("(k p) f -> p k f", p=128), [], [r_wsA[3], r_wsB[3]], "ws3")
            wo = ws[3].rearrange("p k f -> p (k f)").rearrange("p (k f) -> p k f", k=4)
            dma(SP, Ttab, delta_d, [], [r_T], "dlt0")
            dma(SP, ttmp, delta_d, [], [r_obf, r_osq], "dlt1")
            table_steps = [
                lambda: ts(DVE, Ttab, Ttab, 0.0, None, ALU.max, None, [r_T], [r_T]),
                lambda: act(Ttab, Ttab, AF.Exp, [r_T, r_lg], [r_T], scale=lgf),
                lambda: stt(DVE, Ttab, ttmp, 0.0, Ttab, ALU.is_equal, ALU.add, [r_obf, r_osq, r_T], [r_T]),
                lambda: ts(DVE, ttmp, ttmp, 0.0, None, ALU.min, None, [r_obf, r_osq], [r_obf, r_osq]),
                lambda: act(ttmp, ttmp, AF.Exp, [r_obf, r_osq, r_lg], [r_obf, r_osq], scale=nlgb),
                lambda: tt(DVE, Ttab, Ttab, ttmp, ALU.mult, [r_T, r_obf, r_osq], [r_T]),
                lambda: act(decs[:, 0:2], jexp[:, 0:2], AF.Exp, [r_c, r_lg], [r_decs], scale=lgf),
                lambda: act(decs[:, 2:4], jexp[:, 2:4], AF.Exp, [r_c, r_lg], [r_decs], scale=lgb),
                lambda: ts(DVE, decs, decs, 0.0625, None, ALU.mult, None, [r_decs], [r_decs]),
            ]
            for g in range(2):
                base = g * 1024
                cond = g
                isA = (g == 0)
                if isA:
                    for dr in range(2):
                        dma(POOL, S0[:, dr], state_d[jl, dr, hd].rearrange("(c p) e -> p c e", p=128), [], [r_S0[dr]], "S0%d" % dr)
                for tc in range(8):
                    b = pbank()
                    t_ = g * 2 + tc // 4
                    for k in range(8):
                        mm(ps[b][:], hb[:, k, base + tc * 128:base + (tc + 1) * 128], ws[1][:, k, :], k == 0, k == 7,
                           [r_wsA[1], r_wsB[1], r_hb[k][t_]], rps[b])
                    if tc % 2 == 0:
                        act(v[:, tc, :], ps[b][:], AF.Copy, [rps[b]], [r_v[tc]])
                    else:
                        cp(DVE, v[:, tc, :], ps[b][:], [rps[b]], [r_v[tc]])
                    if isA and table_steps:
                        table_steps.pop(0)()
                for c4 in range(4):
                    dest, rdest = (qT, r_qT) if c4 < 2 else (kT, r_kT)
                    c = c4 % 2
                    scl = 1.0 if c4 < 2 else 0.0625
                    rw = r_wsA[0] if c4 < 2 else r_wsB[0]
                    for t2 in range(2):
                        b = pbank()
                        t_ = g * 2 + t2
                        for k in range(8):
                            mm(ps[b][:], ws[0][:, k, c4 * 128:(c4 + 1) * 128], hb[:, k, tsl(t_)], k == 0, k == 7, [rw, r_hb[k][t_]], rps[b])
                        d_ap = dest[:, c, t2 * 512:(t2 + 1) * 512]
                        if not isA:
                            act(d_ap, ps[b][:], AF.Identity, [rps[b]], [rdest[c][t2]], scale=scl)
                        else:
                            qq = pcnt[0] % 2
                            act(qraw[qq], ps[b][:], AF.Identity, [rps[b]], [r_qraw[qq]], scale=scl)
                            b2 = pbank()
                            mm(ps[b2][:], rotb, qraw[qq], True, True, [r_c, r_qraw[qq]], rps[b2])
                            if c == 0:
                                cs = rope[:, 128 + t2 * 8:128 + (t2 + 1) * 8].unsqueeze(2).to_broadcast([128, 8, 64])
                                sn = rope[:, 144 + t2 * 8:144 + (t2 + 1) * 8].unsqueeze(2).to_broadcast([128, 8, 64])
                            else:
                                cs = rope[:, 0:64].unsqueeze(1).to_broadcast([128, 8, 64])
                                sn = rope[:, 64:128].unsqueeze(1).to_broadcast([128, 8, 64])
                            r3 = "p (r c) -> p r c"
                            tt(DVE, t_mean.rearrange(r3, r=8), qraw[qq].rearrange(r3, r=8), cs, ALU.mult, [r_qraw[qq], r_c], [r_st[0]])
                            tt(DVE, t_rstd.rearrange(r3, r=8), ps[b2][:].rearrange(r3, r=8), sn, ALU.mult, [rps[b2], r_c], [r_st[1]])
                            tt(DVE, d_ap, t_mean, t_rstd, ALU.add, [r_st[0], r_st[1]], [rdest[c][t2]])
                while isA and table_steps:
                    table_steps.pop(0)()
                def deferred_proj():
                    for c in range(4):
                        for t2 in range(2):
                            b = pbank()
                            t_ = g * 2 + t2
                            for k in range(8):
                                mm(ps[b][:], ws[2][:, k, c * 128:(c + 1) * 128], hb[:, k, tsl(t_)], k == 0, k == 7,
                                   [r_wsA[2], r_wsB[2], r_hb[k][t_]], rps[b])
                            act(sg[:, c, t2 * 512:(t2 + 1) * 512], ps[b][:], AF.Silu, [rps[b]], [r_sg[c][t2]])
                    if not isA:
                        for tc in range(8):
                            b = pbank()
                            t_ = g * 2 + tc // 4
                            for k in range(8):
                                mm(ps[b][:, 0:256], hb[:, k, base + tc * 128:base + (tc + 1) * 128], ws[0][:, k, 256:512], k == 0, k == 7,
                                   [r_wsB[0], r_hb[k][t_]], rps[b])
                            hf = tc % 2
                            ts(DVE, kdec[:, tc, 0, :], ps[b][:, 0:256], decs[:, hf:hf + 1], None, ALU.mult, None, [rps[b], r_decs], r_kdec)
                            ts(DVE, kdec[:, tc, 1, :], ps[b][:, 0:256], decs[:, 2 + hf:3 + hf], None, ALU.mult, None, [rps[b], r_decs], r_kdec)
                        sc_ = 0
                        for s in range(4):
                            for dr in range(2):
                                for dc in range(2):
                                    b = pbank()
                                    for hf in range(2):
                                        tc = 2 * s + hf
                                        mm(ps[b][:], kdec[:, tc, dr, dc * 128:(dc + 1) * 128], v[:, tc, :], hf == 0, hf == 1,
                                           r_kdec + [r_v[tc]], rps[b])
                                    q = sc_ % 2
                                    sc_ += 1
                                    if q == 0:
                                        act(stage[q], ps[b][:], AF.Copy, [rps[b]], r_stage[q])
                                    else:
                                        cp(DVE, stage[q], ps[b][:], [rps[b]], r_stage[q])
                                    st_outs.append(dma(SP, st_d[s, jl, dr, hd, dc * 128:(dc + 1) * 128, :], stage[q], r_stage[q], [], "stg%d" % q))

                dstate = [True]
                seqs = [(0, 1024, 512)] if isA else [(s * 256, 256, 256) for s in range(4)]
                for (sb0, N, NI) in seqs:
                    NJ = N // 128
                    for it in range(N // NI):
                        i0 = it * NI
                        l0 = sb0 + i0
                        t2 = l0 // 512
                        if isA:
                            for dr in range(2):
                                if dr == 0:
                                    act(rowdec, ip1[:, i0:i0 + NI], AF.Exp, [r_c, r_lg], [r_rowdec], scale=lgf)
                                else:
                                    act(rowdec, ip1[:, i0:i0 + NI], AF.Exp, [r_c, r_lg], [r_rowdec], scale=nlgb,
                                        bias=b1025[:, li + 4 + hd:li + 4 + hd + 1])
                                for dc in range(2):
                                    tt(DVE, qdec[:, dr, dc, :], qT[:, dc, l0:l0 + NI], rowdec, ALU.mult, [r_qT[dc][t2], r_rowdec], [r_qdec[dr]])
                        for jc in range(NJ):
                            b = jc % 2
                            j0 = sb0 + jc * 128
                            for dc in range(2):
                                mm(ps[b][:, 0:NI], kT[:, dc, j0:j0 + 128], qT[:, dc, l0:l0 + NI], dc == 0, dc == 1,
                                   [r_kT[dc][j0 // 512], r_qT[dc][t2]], rps[b])
                            off = i0 - 128 * jc + 896
                            tt(DVE, PT[b][:, 0:NI], ps[b][:, 0:NI], Ttab[:, off:off + NI], ALU.mult, [rps[b], r_T], [r_PT[b]])
                            tcj = j0 // 128
                            for ec in range(4):
                                mm(ps[2 + ec][:, 0:NI], v[:, tcj, ec * 128:(ec + 1) * 128], PT[b][:, 0:NI], jc == 0, (jc == NJ - 1) and not isA,
                                   [r_v[tcj], r_PT[b]], rps[2 + ec])
                        if isA:
                            for dr in range(2):
                                for dc in range(2):
                                    for ec in range(4):
                                        mm(ps[2 + ec][:, 0:NI], S0[:, dr, dc, ec * 128:(ec + 1) * 128], qdec[:, dr, dc, :], False,
                                           dr == 1 and dc == 1, [r_S0[dr], r_qdec[dr]], rps[2 + ec])
                        if dstate[0]:
                            dstate[0] = False
                            deferred_proj()
                        for ec in range(4):
                            act(obf[:, ec, 0:NI], ps[2 + ec][:, 0:NI], AF.Copy, [rps[2 + ec]], [r_obf])
                            act(osq[:, ec, 0:NI], ps[2 + ec][:, 0:NI], AF.Square, [rps[2 + ec]], [r_osq])
                        for ec in range(4):
                            mm(ps[0][:, 0:NI], onesb, obf[:, ec, 0:NI], ec == 0, ec == 3, [r_c, r_obf], rps[0])
                        for ec in range(4):
                            mm(ps[1][:, 0:NI], onesb, osq[:, ec, 0:NI], ec == 0, ec == 3, [r_c, r_osq], rps[1])
                        m_, r_, t1 = t_mean[:, 0:NI], t_rstd[:, 0:NI], t_1[:, 0:NI]
                        ts(DVE, m_, ps[0][:, 0:NI], 1.0 / 512, None, ALU.mult, None, [rps[0]], [r_st[0]])
                        tt(DVE, t1, m_, m_, ALU.mult, [r_st[0]], [r_st[2]])
                        ts(DVE, r_, ps[1][:, 0:NI], 1.0 / 512, LN_EPS, ALU.mult, ALU.add, [rps[1]], [r_st[1]])
                        tt(DVE, r_, r_, t1, ALU.subtract, [r_st[1], r_st[2]], [r_st[1]])
                        act(r_, r_, AF.Sqrt, [r_st[1]], [r_st[1]])
                        P.op(DVE, lambda e, o=r_, i=r_: e.reciprocal(out=o, in_=i), reads=[r_st[1]], writes=[r_st[1]])
                        for ec in range(4):
                            tt(DVE, t1, ps[2 + ec][:, 0:NI], m_, ALU.subtract, [rps[2 + ec], r_st[0]], [r_st[2]])
                            tt(DVE, t1, t1, r_, ALU.mult, [r_st[2], r_st[1]], [r_st[2]])
                            sga = sg[:, ec, l0:l0 + NI]
                            tt(DVE, sga, t1, sga, ALU.mult, [r_st[2], r_sg[ec][t2]], [r_sg[ec][t2]])
                        for dc in range(8):
                            b = pbank()
                            for ec in range(4):
                                mm(ps[b][:, 0:NI], wo[:, ec, dc * 128:(dc + 1) * 128], sg[:, ec, l0:l0 + NI], ec == 0, ec == 3,
                                   [r_wsA[3], r_wsB[3], r_sg[ec][t2]], rps[b])
                            gt = base + l0
                            xs = x[:, dc, gt:gt + NI]
                            stt(DVE, xs, ps[b][:, 0:NI], mod_ap(l, 2, dc, cond), xs, ALU.mult, ALU.add,
                                [rps[b], r_mod, r_x[dc][gt // 512]], [r_x[dc][gt // 512]])

    def conv(l, jl):
        phase_switch()
        zT = av(0, BF16, [128, 8, T]); r_z = [[Res(True) for _ in range(NTT)] for _ in range(8)]
        cgs = [av(32768 + i * 2048, F32, [128, 512]) for i in range(2)]; r_cgs = [Res(True) for _ in range(2)]
        u = [av(36864 + i * 2048, F32, [128, 512]) for i in range(2)]; r_u = [Res(True) for _ in range(2)]
        cu = [av(40960 + i * 2048, F32, [128, 512]) for i in range(2)]; r_cu = [Res(True) for _ in range(2)]
        n = 0
        for half in range(2):
            for i in range(3):
                wdma(i, cvin_d[jl][:, i * 1024 + half * 512:i * 1024 + (half + 1) * 512])
            if half == 0:
                wdma(3, cvout_d[jl][:, 0:512])
            for c in range(4):
                cc = half * 4 + c
                for t_ in range(NTT):
                    q = n % 2
                    n += 1
                    bb = [q, 2 + q, 4 + q]
                    for i in range(3):
                        for k in range(8):
                            mm(ps[bb[i]][:], ws[i][:, k, c * 128:(c + 1) * 128], hb[:, k, tsl(t_)], k == 0, k == 7,
                               [r_wsA[i], r_wsB[i], r_hb[k][t_]], rps[bb[i]])
                    act(cgs[q], ps[bb[1]][:], AF.Copy, [rps[bb[1]]], [r_cgs[q]])
                    tt(DVE, u[q], cgs[q], ps[bb[2]][:], ALU.mult, [r_cgs[q], rps[bb[2]]], [r_u[q]])
                    act(cu[q], u[q], AF.Identity, [r_u[q], r_c], [r_cu[q]], scale=cvw[:, jl, 1, cc:cc + 1])
                    R_ = 8 if t_ < 2 else 2
                    u3 = u[q].rearrange("p (r c) -> p r c", r=R_)
                    c3 = cu[q].rearrange("p (r c) -> p r c", r=R_)
                    stt(DVE, c3[:, :, 1:], u3[:, :, :-1], cvw[:, jl, 0, cc:cc + 1], c3[:, :, 1:], ALU.mult, ALU.add, [r_u[q], r_cu[q], r_c], [r_cu[q]])
                    stt(DVE, c3[:, :, :-1], u3[:, :, 1:], cvw[:, jl, 2, cc:cc + 1], c3[:, :, :-1], ALU.mult, ALU.add, [r_u[q], r_cu[q], r_c], [r_cu[q]])
                    tt(DVE, zT[:, cc, tsl(t_)], ps[bb[0]][:], cu[q], ALU.mult, [rps[bb[0]], r_cu[q]], [r_z[cc][t_]])
        wdma(0, cvout_d[jl][:, 512:1024])
        n = 0
        for dcb in range(2):
            s = 3 if dcb == 0 else 0
            for j in range(4):
                dc = dcb * 4 + j
                for t_ in range(NTT):
                    b = 6 + n % 2
                    n += 1
                    for k in range(8):
                        mm(ps[b][:], ws[s][:, k, j * 128:(j + 1) * 128], zT[:, k, tsl(t_)], k == 0, k == 7,
                           [r_wsA[s], r_wsB[s], r_z[k][t_]], rps[b])
                    cond = 0 if t_ < 2 else 1
                    xs = x[:, dc, tsl(t_)]
                    stt(DVE, xs, ps[b][:], mod_ap(l, 2, dc, cond), xs, ALU.mult, ALU.add, [rps[b], r_mod, r_x[dc][t_]], [r_x[dc][t_]])

    def moe(l):
        phase_switch()
        hidb = [av(0, BF16, [128, 4, T]), av(32768, BF16, [128, 4, T])]
        r_hidb = [[[Res(True) for _ in range(NTT)] for _ in range(4)] for _ in range(2)]
        sgt = [av(16384 + i * 2048, F32, [128, 512]) for i in range(2)]; r_sgt = [Res(True) for _ in range(2)]
        tm = [av(20480 + i * 2048, F32, [128, 512]) for i in range(2)]; r_tm = [Res(True) for _ in range(2)]
        cHi = av(24576, BF16, [128, T]); cLo = av(28672, BF16, [128, T]); r_combT = [Res(True) for _ in range(NTT)]
        o = 32768
        SZ = 16 * E * 4
        scores = av(o, F32, [128, 16, E]); r_sc = Res(True)
        sel = av(o + SZ, F32, [128, 16, E]); r_sel = Res(True)
        tA = av(o + 2 * SZ, F32, [128, 16, E]); r_tA = Res(True)
        comb = av(o + 3 * SZ, F32, [128, 16, E]); r_comb = Res(True)
        o2 = o + 4 * SZ
        m1 = av(o2, F32, [128, 128]); m2 = av(o2 + 512, F32, [128, 128]); grp = av(o2 + 1024, F32, [128, 128])
        top8 = av(o2 + 1536, F32, [128, 16, 8]); gm = av(o2 + 2048, F32, [128, 128]); top8e = av(o2 + 2560, F32, [128, 16, 8])
        den = av(o2 + 3072, F32, [128, 16])
        r_sm = Res(True)

        dma(POOL, wr, rout_d[l].rearrange("(k p) f -> p k f", p=128), [], [r_wr], "wr")
        for tc in range(16):
            b = 6 + tc // 8
            t_ = tc // 4
            for k in range(8):
                mm(ps[b][:, (tc % 8) * E:(tc % 8 + 1) * E], hb[:, k, tc * 128:(tc + 1) * 128], wr[:, k, :], k == 0, k == 7,
                   [r_wr, r_hb[k][t_]], rps[b], acc=not (tc % 8 == 0 and k == 0))
        for b_ in range(2):
            act(scores[:, b_ * 8:(b_ + 1) * 8, :].rearrange("p a e -> p (a e)"), ps[6 + b_][:, 0:8 * E], AF.Sigmoid, [rps[6 + b_]], [r_sc])
        g3 = "p a (g e) -> p (a g) e"
        tt(DVE, sel, scores, mbias[:, l, :].unsqueeze(1).to_broadcast([128, 16, E]), ALU.add, [r_sc, r_c], [r_sel])
        selg = sel.rearrange(g3, g=8)
        tAg = tA.rearrange(g3, g=8)
        P.op(DVE, lambda e: e.tensor_reduce(out=m1, in_=selg, axis=AX.X, op=ALU.max), reads=[r_sel], writes=[r_sm])
        tt(DVE, tAg, selg, m1.unsqueeze(2).to_broadcast([128, 128, PG]), ALU.is_equal, [r_sel, r_sm], [r_tA])
        stt(DVE, tA, tA, -1e9, sel, ALU.mult, ALU.add, [r_tA, r_sel], [r_tA])
        P.op(DVE, lambda e: e.tensor_reduce(out=m2, in_=tAg, axis=AX.X, op=ALU.max), reads=[r_tA], writes=[r_sm])
        tt(DVE, grp, m1, m2, ALU.add, [r_sm], [r_sm])
        for tc in range(16):
            P.op(DVE, lambda e, tc=tc: e.max(out=top8[:, tc, :], in_=grp[:, tc * 8:(tc + 1) * 8]), reads=[r_sm], writes=[r_sm])
        tt(DVE, gm.rearrange("p (a g) -> p a g", a=16), grp.rearrange("p (a g) -> p a g", a=16),
           top8[:, :, 3:4].to_broadcast([128, 16, 8]), ALU.is_ge, [r_sm], [r_sm])
        ts(DVE, tA, sel, 2.0, None, ALU.add, None, [r_sel], [r_tA])
        tt(DVE, tAg, tAg, gm.unsqueeze(2).to_broadcast([128, 128, PG]), ALU.mult, [r_tA, r_sm], [r_tA])
        for tc in range(16):
            P.op(DVE, lambda e, tc=tc: e.max(out=top8e[:, tc, :], in_=tA[:, tc, :]), reads=[r_tA], writes=[r_sm])
        tt(DVE, sel, tA, top8e[:, :, 5:6].to_broadcast([128, 16, E]), ALU.is_ge, [r_tA, r_sm], [r_sel])
        tt(DVE, tA, scores, sel, ALU.mult, [r_sc, r_sel], [r_tA])
        P.op(DVE, lambda e: e.tensor_reduce(out=den, in_=tA, axis=AX.X, op=ALU.add), reads=[r_tA], writes=[r_sm])
        P.op(DVE, lambda e: e.reciprocal(out=den, in_=den), reads=[r_sm], writes=[r_sm])
        ts(DVE, den, den, 2.5, None, ALU.mult, None, [r_sm], [r_sm])
        tt(DVE, comb, tA, den.unsqueeze(2).to_broadcast([128, 16, E]), ALU.mult, [r_tA, r_sm], [r_comb])
        for tc in range(16):
            b = 6 + (tc // 4) % 2
            tr(ps[b][0:E, (tc % 4) * 128:(tc % 4 + 1) * 128], comb[:, tc, :], [r_comb], rps[b])
            if tc % 4 == 3:
                t_ = tc // 4
                act(cHi[0:E, tsl(t_)], ps[b][0:E, :], AF.Copy, [rps[b]], [r_combT[t_]])
                tt(DVE, cLo[0:E, tsl(t_)], ps[b][0:E, :], cHi[0:E, tsl(t_)], ALU.subtract, [rps[b], r_combT[t_]], [r_combT[t_]])

        NX = E + 1
        wdA = [av(52288, BF16, [128, 2, 1024]), av(52288 + 4096, BF16, [128, 2, 1024])]
        wd4 = [wdS[0], wdS[1], wdA[0], wdA[1]]
        r_wd4 = [r_wd[0], r_wd[1], Res(True), Res(True)]
        router_res = [r_sc, r_sel, r_tA, r_comb, r_sm]
        pairs = [[e for e in (2 * p, 2 * p + 1) if e < NX] for p in range((NX + 1) // 2)]

        def load_gu(e):
            s = e % 4
            if e < E:
                wdma(s, wg_d[l, e], half=0)
                wdma(s, wu_d[l, e], half=1)
            else:
                wdma(s, sg_d[l], half=0)
                wdma(s, su_d[l], half=1)

        def load_d(p):
            for e in pairs[p]:
                s = (p % 2) * 2 + e % 2
                src = wd_d[l, e] if e < E else sd_d[l]
                dma(POOL, wd4[s], src.rearrange("(k p) f -> p k f", p=128), [], [r_wd4[s]], "wd%d" % s)

        cnt = [0]
        dn = [0]
        first_b = [True] * 16

        def compute_block(p, e, t_):
            s = e % 4
            m = e % 2
            hid, r_hid = hidb[p % 2], r_hidb[p % 2]
            bcb = 4 if t_ % 2 == 0 else 6
            if e < E:
                sel1 = identb[0:E, e:e + 1].to_broadcast([E, 128])
                mm(ps[bcb][:], sel1, cHi[0:E, tsl(t_)], True, False, [r_c, r_combT[t_]], rps[bcb])
                mm(ps[bcb][:], sel1, cLo[0:E, tsl(t_)], False, True, [r_c, r_combT[t_]], rps[bcb])
            for fc in range(2):
                q = cnt[0] % 2
                cnt[0] += 1
                for k in range(8):
                    mm(ps[q][:], ws[s][:, k, fc * 128:(fc + 1) * 128], hb[:, k, tsl(t_)], k == 0, k == 7, [r_wsA[s], r_hb[k][t_]], rps[q])
                for k in range(8):
                    mm(ps[2 + q][:], ws[s][:, k, 256 + fc * 128:256 + (fc + 1) * 128], hb[:, k, tsl(t_)], k == 0, k == 7,
                       [r_wsB[s], r_hb[k][t_]], rps[2 + q])
                act(sgt[q], ps[q][:], AF.Silu, [rps[q]], [r_sgt[q]])
                hd_ = hid[:, m * 2 + fc, tsl(t_)]
                wres = [r_hid[m * 2 + fc][t_]]
                if p % 2 == 1 and first_b[(m * 2 + fc) * 4 + t_]:
                    first_b[(m * 2 + fc) * 4 + t_] = False
                    wres = wres + router_res
                if e < E:
                    tt(DVE, tm[q], sgt[q], ps[2 + q][:], ALU.mult, [r_sgt[q], rps[2 + q]], [r_tm[q]])
                    tt(DVE, hd_, tm[q], ps[bcb][:], ALU.mult, [r_tm[q], rps[bcb]], wres)
                else:
                    tt(DVE, hd_, sgt[q], ps[2 + q][:], ALU.mult, [r_sgt[q], rps[2 + q]], wres)

        def down_group(p, t_, dc):
            members = pairs[p]
            hid, r_hid = hidb[p % 2], r_hidb[p % 2]
            cond = 0 if t_ < 2 else 1
            b = 5 + dn[0] % 2 * 2
            dn[0] += 1
            n_ = len(members) * 2
            i = 0
            for e in members:
                m = e % 2
                s = (p % 2) * 2 + m
                for fc in range(2):
                    mm(ps[b][:], wd4[s][:, fc, dc * 128:(dc + 1) * 128], hid[:, m * 2 + fc, tsl(t_)], i == 0, i == n_ - 1,
                       [r_wd4[s], r_hid[m * 2 + fc][t_]], rps[b])
                    i += 1
            xs = x[:, dc, tsl(t_)]
            stt(DVE, xs, ps[b][:], mod_ap(l, 5, dc, cond), xs, ALU.mult, ALU.add, [rps[b], r_mod, r_x[dc][t_]], [r_x[dc][t_]])

        PF = 2
        for e in range(min(PF, NX)):
            load_gu(e)
        load_d(0)
        if len(pairs) > 1:
            load_d(1)
        pending = []
        for p, members in enumerate(pairs):
            nslots = len(members) * NTT
            per = -(-len(pending) // nslots) if pending else 0
            for e in members:
                if e + PF < NX:
                    load_gu(e + PF)
                for t_ in range(NTT):
                    compute_block(p, e, t_)
                    for _ in range(per):
                        if pending:
                            pp, tq, dq = pending.pop(0)
                            down_group(pp, tq, dq)
            while pending:
                pp, tq, dq = pending.pop(0)
                down_group(pp, tq, dq)
            if p >= 1 and p + 1 < len(pairs):
                load_d(p + 1)
            pending = [(p, tq, dq) for tq in range(NTT) for dq in range(8)]
        while pending:
            pp, tq, dq = pending.pop(0)
            down_group(pp, tq, dq)

    st_outs = []
    make_hb(0, 1, 0, [DVE, POOL])
    for k in range(8):
        for t_ in range(NTT):
            ts(POOL, x[:, k, tsl(t_)], x[:, k, tsl(t_)], ALPHA, None, ALU.mult, None, [r_x[k][t_]], [r_x[k][t_]])
    done = False
    for l in range(L):
        jl = l // 2
        if l > 0:
            make_hb(l, 1, 0, [DVE, POOL])
        if l % 2 == 0:
            retention(l, jl)
        else:
            conv(l, jl)
        if stop_after == (l, "mix"):
            break
        layer_norm(l, 0, True)
        if stop_after == (l, "ln1"):
            break
        moe(l)
        if stop_after == (l, "moe"):
            break
        layer_norm(l, 1, False)
    outs = []
    for k in range(8):
        outs.append(dma(SP, yT_d[k * 128:(k + 1) * 128, :], x[:, k, :], r_x[k], [], "out%d" % k))
    P.emit(final_waits=outs + st_outs[-2:])
    return nc, P


def const_inputs():
    p = np.arange(128)
    ident = np.eye(128, dtype=np.float32)
    rot = np.zeros((128, 128), np.float32)
    for m in range(64):
        rot[m + 64, m] = -1.0
        rot[m, m + 64] = 1.0
    delta = (np.arange(1920)[None, :] - p[:, None] - 896).astype(np.float32)
    ip1 = np.broadcast_to(np.arange(1, 1025, dtype=np.float32)[None, :], (128, 1024)).copy()
    jexp = np.stack([255.0 - p, 255.0 - (128 + p), 0.0 + p, 128.0 + p], axis=1).astype(np.float32)
    freqs = (10000.0 ** (-np.arange(64, dtype=np.float32) / 64)).astype(np.float32)
    fr = freqs[p % 64]
    angC = np.arange(64, dtype=np.float32)[None, :] * fr[:, None]
    angR = np.arange(16, dtype=np.float32)[None, :] * fr[:, None]
    rope = np.concatenate([np.cos(angC), np.sin(angC), np.cos(angR), np.sin(angR)], axis=1).astype(np.float32)
    return {"ident": ident, "rotm": rot, "delta": delta, "ip1": ip1, "jexp": jexp, "rope": rope}


def per_core_inputs(inp, core, L, E):
    NR, NCV = (L + 1) // 2, L // 2
    f = np.float32
    xs = np.asarray(inp["x_sample"][core], f)
    xp = np.asarray(inp["x_prompt"][4 * core:4 * core + 4], f).reshape(1024, 1024)
    xT = np.ascontiguousarray(np.concatenate([xs, xp], axis=0).T)
    cond = np.stack([np.asarray(inp["c"][core], f), np.asarray(inp["c_ctx"], f)], axis=1)
    condT = np.ascontiguousarray(cond.reshape(8, 128, 2).transpose(1, 0, 2))
    d = {"xT": xT, "condT": condT, "state": np.ascontiguousarray(np.asarray(inp["state_ret"][core], f))}
    return d


def shared_inputs(inp, L, E):
    NR, NCV = (L + 1) // 2, L // 2
    f = np.float32
    d = {}
    for k in ("ada_w", "ret_w_in", "ret_w_out", "moe_router", "moe_w_gate", "moe_w_up", "moe_w_down",
              "shared_w_gate", "shared_w_up", "shared_w_down"):
        d[k] = np.ascontiguousarray(np.asarray(inp[k], f))
    if NCV:
        d["conv_w_in"] = np.ascontiguousarray(np.asarray(inp["conv_w_in"], f))
        d["conv_w_out"] = np.ascontiguousarray(np.asarray(inp["conv_w_out"], f))
        cw = np.asarray(inp["conv_w"], f)
        d["convw"] = np.ascontiguousarray(cw.reshape(NCV, 3, 8, 128).transpose(3, 0, 1, 2))
    ab = np.asarray(inp["ada_b"], f).reshape(L, 48, 128).transpose(2, 0, 1)
    d["adab"] = np.ascontiguousarray(np.repeat(ab[:, :, :, None], 2, axis=3))
    g = np.asarray(inp["ln_g"], f).reshape(L, 2, 8, 128)
    b = np.asarray(inp["ln_b"], f).reshape(L, 2, 8, 128)
    d["lnp"] = np.ascontiguousarray(np.stack([g, b], axis=2).transpose(4, 0, 1, 2, 3))
    d["rdecay"] = np.ascontiguousarray(np.broadcast_to(np.asarray(inp["ret_decay"], f).reshape(1, NR * 8), (128, NR * 8)))
    d["mbias"] = np.ascontiguousarray(np.broadcast_to(np.asarray(inp["moe_bias"], f)[None], (128, L, E)))
    d.update(const_inputs())
    return d


_CACHE = {}


def run(inp, n_cores, L=4, E=64, stop_after=None, trace=False):
    key = (L, E, stop_after)
    if key not in _CACHE:
        _CACHE[key] = build(L, E, stop_after)
    nc, P = _CACHE[key]
    sh = shared_inputs(inp, L, E)
    in_maps = []
    for c in range(n_cores):
        m = dict(sh)
        m.update(per_core_inputs(inp, c, L, E))
        in_maps.append(m)
    res = run_bass_kernel_spmd(nc, in_maps, core_ids=list(range(n_cores)), trace=trace)
    return res


def kernel(**inputs):
    res = run(inputs, 8)
    NR = 2
    y_s = np.empty((8, 1024, 1024), np.float32)
    y_p = np.empty((32, 256, 1024), np.float32)
    st = np.empty((32, NR, 2, 4, 256, 512), np.float32)
    for c in range(8):
        yT = np.asarray(res.results[c]["yT"])
        y = yT.T
        y_s[c] = y[0:1024]
        y_p[4 * c:4 * c + 4] = y[1024:2048].reshape(4, 256, 1024)
        st[4 * c:4 * c + 4] = np.asarray(res.results[c]["st"])
    return (y_p, y_s, st)
```

```python
import numpy as np
import concourse.bass as bass
import concourse.mybir as mybir
from concourse.bass_utils import run_bass_kernel_spmd

F32 = mybir.dt.float32
BF16 = mybir.dt.bfloat16
AF = mybir.ActivationFunctionType
ALU = mybir.AluOpType
AX = mybir.AxisListType

PE, ACT, DVE, POOL, SP = "tensor", "scalar", "vector", "gpsimd", "sync"
ALPHA = (2.0 * 4) ** 0.25
LN_EPS = 1e-5
T = 2048
NTT = 4


class Res:
    __slots__ = ("last_w", "readers", "arena", "psum")

    def __init__(self, arena=False, psum=False):
        self.last_w = None
        self.readers = []
        self.arena = arena
        self.psum = psum


class Op:
    __slots__ = ("eng", "fn", "deps", "signal", "semkey", "sigval", "is_dma")

    def __init__(self, eng, fn, is_dma, semkey):
        self.eng, self.fn, self.is_dma, self.semkey = eng, fn, is_dma, semkey
        self.deps, self.signal, self.sigval = (), False, None


class Prog:
    def __init__(self, nc):
        self.nc = nc
        self.ops = {e: [] for e in (PE, ACT, DVE, POOL, SP)}
        self.all_ops = []
        self.arena_tok = Res()

    def op(self, eng, fn, reads=(), writes=(), is_dma=False, semkey=None, pe_acc=False):
        o = Op(eng, fn, is_dma, semkey)
        reads = list(reads)
        if any(r.arena for r in reads) or any(w.arena for w in writes):
            reads.append(self.arena_tok)
        deps = set()
        for r in reads:
            if r.last_w is not None:
                deps.add(r.last_w)
            if r.psum:
                for rd in r.readers:
                    if rd.eng != eng:
                        deps.add(rd)
        for w in writes:
            if w.readers:
                deps.update(w.readers)
            elif w.last_w is not None:
                if not (pe_acc and eng == PE and w.last_w.eng == PE and not w.last_w.is_dma):
                    deps.add(w.last_w)
        for r in reads:
            r.readers.append(o)
        for w in writes:
            w.last_w = o
            w.readers = []
        deps.discard(o)
        o.deps = tuple(deps)
        self.ops[eng].append(o)
        self.all_ops.append(o)
        return o

    def emit(self, final_waits=()):
        nc = self.nc

        def qkey(o):
            return ("dma", o.semkey) if o.is_dma else o.eng

        order, cnt = {}, {}
        for o in self.all_ops:
            k = qkey(o)
            cnt[k] = cnt.get(k, 0) + 1
            order[o] = cnt[k]
        needed = {}
        for eng in self.ops:
            seen = {}
            for o in self.ops[eng]:
                best = {}
                for d in o.deps:
                    k = qkey(d)
                    od = order[d]
                    if od > seen.get(k, 0) and od > best.get(k, (0, None))[0]:
                        best[k] = (od, d)
                lst = []
                for k, (od, d) in best.items():
                    seen[k] = od
                    lst.append(d)
                    d.signal = True
                needed[o] = lst
        for d in final_waits:
            d.signal = True
        sems, sigcount = {}, {}
        for o in self.all_ops:
            if o.signal:
                k = qkey(o)
                if k not in sems:
                    sems[k] = nc.alloc_semaphore(name="s%d" % len(sems))
                sigcount[k] = sigcount.get(k, 0) + (16 if o.is_dma else 1)
                o.sigval = sigcount[k]
        self.n_sems = len(sems)
        self.sigcount = sigcount
        with nc.Block() as block:
            def mk(eng_name):
                def body(eng):
                    for o in self.ops[eng_name]:
                        for d in needed[o]:
                            eng.wait_ge(sems[qkey(d)], d.sigval)
                        ins = o.fn(eng)
                        if o.signal:
                            ins.then_inc(sems[qkey(o)], 16 if o.is_dma else 1)
                    if eng_name == SP:
                        for d in final_waits:
                            eng.wait_ge(sems[qkey(d)], d.sigval)
                return body
            block.sync(mk(SP))
            block.tensor(mk(PE))
            block.scalar(mk(ACT))
            block.vector(mk(DVE))
            block.gpsimd(mk(POOL))


def build(L=4, E=64, stop_after=None):
    NR, NCV = (L + 1) // 2, L // 2
    PG = E // 8
    nc = bass.Bass("TRN2", target_bir_lowering=False)
    P = Prog(nc)

    def din(name, shape, dt=F32):
        return nc.dram_tensor(name, list(shape), dt, kind="ExternalInput").ap()

    xT_d = din("xT", [1024, T])
    cond_d = din("condT", [128, 8, 2])
    state_d = din("state", [NR, 2, 4, 256, 512])
    adaw_d = din("ada_w", [L, 1024, 6144])
    adab_d = din("adab", [128, L, 48, 2])
    lnp_d = din("lnp", [128, L, 2, 2, 8])
    retin_d = din("ret_w_in", [NR, 1024, 6144])
    retout_d = din("ret_w_out", [NR, 2048, 1024])
    rdec_d = din("rdecay", [128, NR * 8])
    if NCV:
        cvin_d = din("conv_w_in", [NCV, 1024, 3072])
        cvw_d = din("convw", [128, NCV, 3, 8])
        cvout_d = din("conv_w_out", [NCV, 1024, 1024])
    rout_d = din("moe_router", [L, 1024, E])
    mbias_d = din("mbias", [128, L, E])
    wg_d = din("moe_w_gate", [L, E, 1024, 256])
    wu_d = din("moe_w_up", [L, E, 1024, 256])
    wd_d = din("moe_w_down", [L, E, 256, 1024])
    sg_d = din("shared_w_gate", [L, 1024, 256])
    su_d = din("shared_w_up", [L, 1024, 256])
    sd_d = din("shared_w_down", [L, 256, 1024])
    ident_d = din("ident", [128, 128])
    rot_d = din("rotm", [128, 128])
    delta_d = din("delta", [128, 1920])
    ip1_d = din("ip1", [128, 1024])
    jexp_d = din("jexp", [128, 4])
    rope_d = din("rope", [128, 160])
    yT_d = nc.dram_tensor("yT", [1024, T], F32, kind="ExternalOutput").ap()
    st_d = nc.dram_tensor("st", [4, NR, 2, 4, 256, 512], F32, kind="ExternalOutput").ap()

    SB_BYTES = 211840
    sb = nc.alloc_sbuf_tensor("sb", [128, SB_BYTES // 2], BF16)
    cur = [0]

    def view(off, dt, shape):
        n = int(np.prod(shape[1:])) * (4 if dt == F32 else 2)
        a = sb[:, off // 2:(off + n) // 2]
        if dt == F32:
            a = a.bitcast(F32)
        if len(shape) == 3:
            a = a.rearrange("p (a b) -> p a b", a=shape[1])
        elif len(shape) == 4:
            a = a.rearrange("p (a b c) -> p a b c", a=shape[1], b=shape[2])
        return a

    def alloc(dt, shape):
        n = int(np.prod(shape[1:])) * (4 if dt == F32 else 2)
        n = (n + 31) // 32 * 32
        off = cur[0]
        cur[0] += n
        return view(off, dt, shape)

    x = alloc(F32, [128, 8, T])
    hb = alloc(BF16, [128, 8, T])
    ws = [alloc(BF16, [128, 8, 512]) for _ in range(4)]
    WD_OFF = cur[0]
    wdS = [alloc(BF16, [128, 2, 1024]) for _ in range(2)]
    ada_slot = view(WD_OFF, BF16, [128, 8, 512])
    ident = alloc(F32, [128, 128])
    ones32 = alloc(F32, [128, 128])
    onesb = alloc(BF16, [128, 128])
    rot32 = None
    rotb = alloc(BF16, [128, 128])
    identb = alloc(BF16, [128, 128])
    mod = alloc(F32, [128, L, 48, 2])
    lnp = alloc(F32, [128, L, 2, 2, 8][0:1] + [L * 32])
    lnp = lnp.rearrange("p (l a b k) -> p l a b k", l=L, a=2, b=2)
    if NCV:
        cvw = alloc(F32, [128, NCV * 24]).rearrange("p (j c k) -> p j c k", j=NCV, c=3)
    mbias = alloc(F32, [128, L, E])
    lg = alloc(F32, [128, NR * 8])
    nlg = alloc(F32, [128, NR * 8])
    b1025 = alloc(F32, [128, NR * 8])
    lgtmp = alloc(F32, [128, NR * 8])
    decs = alloc(F32, [128, 4])
    jexp = alloc(F32, [128, 4])
    rope = alloc(F32, [128, 160])
    condT = alloc(F32, [128, 8, 2])
    scT = alloc(BF16, [128, 8, 2])
    ip1 = alloc(F32, [128, 1024])
    wr = alloc(BF16, [128, 8, E])
    ARENA0 = cur[0]
    ARENA = SB_BYTES - ARENA0
    assert ARENA >= 60928, ARENA

    def av(off, dt, shape):
        assert off + int(np.prod(shape[1:])) * (4 if dt == F32 else 2) <= ARENA, (off, shape)
        return view(ARENA0 + off, dt, shape)

    ps = [nc.alloc_psum_tensor("ps%d" % i, [128, 512], F32) for i in range(8)]
    rps = [Res(psum=True) for _ in range(8)]

    r_x = [[Res() for _ in range(NTT)] for _ in range(8)]
    r_hb = [[Res() for _ in range(NTT)] for _ in range(8)]
    r_wsA = [Res() for _ in range(4)]
    r_wsB = [Res() for _ in range(4)]
    r_wd = [Res() for _ in range(2)]
    r_c = Res()
    r_modl = [Res() for _ in range(L)]
    r_lg = Res()
    r_decs = Res()
    r_wr = Res()
    r_scT = Res()

    def mm(out, lhsT, rhs, start, stop, R, W, acc=None):
        P.op(PE, lambda e: e.matmul(out, lhsT=lhsT, rhs=rhs, start=start, stop=stop), reads=R, writes=[W],
             pe_acc=(not start) if acc is None else acc)

    def tr(out, in_, R, W):
        P.op(PE, lambda e: e.transpose(out, in_, ident), reads=R + [r_c], writes=[W], pe_acc=True)

    def act(out, in_, func, R, W, scale=1.0, bias=0.0):
        P.op(ACT, lambda e: e.activation(out=out, in_=in_, func=func, bias=bias, scale=scale), reads=R, writes=W)

    def tt(eng, out, in0, in1, op, R, W):
        P.op(eng, lambda e: e.tensor_tensor(out=out, in0=in0, in1=in1, op=op), reads=R, writes=W)

    def ts(eng, out, in0, s1, s2, op0, op1, R, W):
        if op1 is None:
            P.op(eng, lambda e: e.tensor_scalar(out=out, in0=in0, scalar1=s1, scalar2=None, op0=op0), reads=R, writes=W)
        else:
            P.op(eng, lambda e: e.tensor_scalar(out=out, in0=in0, scalar1=s1, scalar2=s2, op0=op0, op1=op1), reads=R, writes=W)

    def stt(eng, out, in0, scalar, in1, op0, op1, R, W):
        P.op(eng, lambda e: e.scalar_tensor_tensor(out=out, in0=in0, scalar=scalar, in1=in1, op0=op0, op1=op1), reads=R, writes=W)

    def cp(eng, out, in_, R, W):
        P.op(eng, lambda e: e.tensor_copy(out=out, in_=in_), reads=R, writes=W)

    dma_n = [0]

    def dma(eng, out, in_, R, W, key):
        return P.op(eng, lambda e: e.dma_start(out=out, in_=in_), reads=R, writes=W, is_dma=True, semkey=key)

    def wdma(slot, src, half=None):
        s = src.rearrange("(k p) f -> p k f", p=128)
        if half is None:
            dma(POOL, ws[slot], s, [], [r_wsA[slot], r_wsB[slot]], "ws%d" % slot)
        elif half == 0:
            dma(POOL, ws[slot][:, :, 0:256], s, [], [r_wsA[slot]], "wsA%d" % slot)
        else:
            dma(POOL, ws[slot][:, :, 256:512], s, [], [r_wsB[slot]], "wsB%d" % slot)

    def phase_switch():
        P.op(POOL, lambda e: e.memset(decs[:, 0:1], 0.0), reads=[], writes=[P.arena_tok, r_decs])

    def tsl(t_):
        return slice(t_ * 512, (t_ + 1) * 512)

    dma(SP, ident, ident_d, [], [r_c], "c0")
    dma(SP, ip1, ip1_d, [], [r_c], "c2")
    dma(SP, jexp, jexp_d, [], [r_c], "c3")
    dma(SP, rope, rope_d, [], [r_c], "c4")
    dma(SP, condT, cond_d, [], [r_c], "c5")
    dma(SP, mod, adab_d, [], r_modl, "c6")
    dma(SP, lnp, lnp_d, [], [r_c], "c7")
    dma(SP, mbias, mbias_d, [], [r_c], "c8")
    dma(SP, lg, rdec_d, [], [r_lg], "c9")
    if NCV:
        dma(SP, cvw, cvw_d, [], [r_c], "c10")
    dma(POOL, rotb, rot_d, [], [r_c], "c11")
    dma(POOL, identb, ident_d, [], [r_c], "c12")
    for k in range(8):
        dma(SP, x[:, k, :], xT_d[k * 128:(k + 1) * 128, :], [], r_x[k], "x%d" % k)
    P.op(DVE, lambda e: e.memset(ones32, 1.0), writes=[r_c])
    P.op(DVE, lambda e: e.memset(onesb, 1.0), writes=[r_c])
    act(lgtmp, lg, AF.Exp, [r_lg], [r_lg], scale=-1.0)
    ts(DVE, lgtmp, lgtmp, 1.0, None, ALU.add, None, [r_lg], [r_lg])
    act(lgtmp, lgtmp, AF.Ln, [r_lg], [r_lg])
    ts(DVE, lg, lgtmp, -1.0, None, ALU.mult, None, [r_lg], [r_lg])
    cp(DVE, nlg, lgtmp, [r_lg], [r_lg])
    ts(DVE, b1025, lg, 1025.0, None, ALU.mult, None, [r_lg], [r_lg])
    act(scT, condT, AF.Silu, [r_c], [r_scT])
    for l in range(1):
        for blk in range(12):
            s = blk % 4
            wdma(s, adaw_d[l][:, blk * 512:(blk + 1) * 512])
            for j in range(4):
                oc = blk * 4 + j
                for k in range(8):
                    mm(ps[7][:, oc * 2:oc * 2 + 2], ws[s][:, k, j * 128:(j + 1) * 128], scT[:, k, :], k == 0, k == 7,
                       [r_wsA[s], r_wsB[s], r_scT], rps[7], acc=not (blk == 0 and j == 0 and k == 0))
        tt(DVE, mod[:, l].rearrange("p a b -> p (a b)"), mod[:, l].rearrange("p a b -> p (a b)"), ps[7][:, 0:96], ALU.add,
           [rps[7], r_modl[l]], [r_modl[l]])
        for base in (8, 32):
            f = 1.0 if (l == 0 and base == 8) else 1.0 / ALPHA
            v = mod[:, l, base:base + 8, :]
            ts(DVE, v, v, 1.0, f, ALU.add, ALU.mult, [r_modl[l]], [r_modl[l]])
    for l in range(L):
        for w in range(2):
            if not (l == L - 1 and w == 1):
                v = lnp[:, l, w]
                ts(DVE, v, v, ALPHA, None, ALU.mult, None, [r_c], [r_c])

    bg = [None]

    def ada_bg_start(l):
        bg[0] = {"l": l, "next": 0, "loaded": None}

    def ada_bg_step(bank_fn):
        st_ = bg[0]
        if st_ is None:
            return
        l = st_["l"]
        if st_["loaded"] is not None:
            blk = st_["loaded"]
            b = bank_fn()
            for j in range(4):
                for k in range(8):
                    mm(ps[b][:, j * 2:j * 2 + 2], ada_slot[:, k, j * 128:(j + 1) * 128], scT[:, k, :], k == 0, k == 7,
                       r_wd + [r_scT], rps[b], acc=not (j == 0 and k == 0))
            mv = mod[:, l, blk * 4:(blk + 1) * 4, :].rearrange("p a b -> p (a b)")
            tt(DVE, mv, mv, ps[b][:, 0:8], ALU.add, [rps[b], r_modl[l]], [r_modl[l]])
            st_["loaded"] = None
        if st_["next"] < 12:
            blk = st_["next"]
            dma(POOL, ada_slot, adaw_d[l][:, blk * 512:(blk + 1) * 512].rearrange("(k p) f -> p k f", p=128), [], r_wd, "adaslot")
            st_["loaded"] = blk
            st_["next"] += 1
        else:
            for base in (8, 32):
                v = mod[:, l, base:base + 8, :]
                ts(DVE, v, v, 1.0, 1.0 / ALPHA, ALU.add, ALU.mult, [r_modl[l]], [r_modl[l]])
            bg[0] = None

    def ada_bg_finish(bank_fn):
        while bg[0] is not None:
            ada_bg_step(bank_fn)

    def mod_ap(l, which, k, cond):
        return mod[:, l, which * 8 + k, cond:cond + 1]

    def make_hb(l, which_sc, which_sh, eng_list):
        i = 0
        for k in range(8):
            for t_ in range(NTT):
                cond = 0 if t_ < 2 else 1
                ts(eng_list[i % len(eng_list)], hb[:, k, tsl(t_)], x[:, k, tsl(t_)], mod_ap(l, which_sc, k, cond), mod_ap(l, which_sh, k, cond),
                   ALU.mult, ALU.add, [r_x[k][t_], r_modl[l]], [r_hb[k][t_]])
                i += 1

    def accum_x(psum_ap, l, which_g, dc, tok0, n):
        t_ = tok0 // 512
        cond = 0 if t_ < 2 else 1
        xs = x[:, dc, tok0:tok0 + n]
        stt(DVE, xs, psum_ap, mod_ap(l, which_g, dc, cond), xs, ALU.mult, ALU.add, None, None)

    def layer_norm(l, w, then_hb):
        phase_switch()
        S_ = [[av((i * 4 + t_) * 2048, F32, [128, 512]) for t_ in range(NTT)] for i in range(3)]
        r_S = [[Res(True) for _ in range(NTT)] for _ in range(3)]
        tmp = [av(24576 + i * 2048, F32, [128, 512]) for i in range(4)]
        r_tmp = [Res(True) for _ in range(4)]
        xsq = [av(32768 + i * 1024, BF16, [128, 512]) for i in range(4)]
        r_xsq = [Res(True) for _ in range(4)]
        xbf = [av(36864 + i * 1024, BF16, [128, 512]) for i in range(4)]
        r_xbf = [Res(True) for _ in range(4)]
        n = 0
        for t_ in range(NTT):
            b1, b2 = 2 * t_, 2 * t_ + 1
            for k in range(8):
                q = n % 4
                n += 1
                act(xsq[q], x[:, k, tsl(t_)], AF.Square, [r_x[k][t_]], [r_xsq[q]])
                mm(ps[b2][:], onesb, xsq[q], k == 0, k == 7, [r_c, r_xsq[q]], rps[b2])
                cp(DVE, xbf[q], x[:, k, tsl(t_)], [r_x[k][t_]], [r_xbf[q]])
                mm(ps[b1][:], onesb, xbf[q], k == 0, k == 7, [r_c, r_xbf[q]], rps[b1])
        for t_ in range(NTT):
            b1, b2 = 2 * t_, 2 * t_ + 1
            mean, tv, msq = S_[0][t_], S_[1][t_], S_[2][t_]
            ts(DVE, mean, ps[b1][:], 1.0 / 1024, None, ALU.mult, None, [rps[b1]], [r_S[0][t_]])
            tt(DVE, msq, mean, mean, ALU.mult, [r_S[0][t_]], [r_S[2][t_]])
            ts(DVE, tv, ps[b2][:], 1.0 / 1024, LN_EPS, ALU.mult, ALU.add, [rps[b2]], [r_S[1][t_]])
            tt(DVE, tv, tv, msq, ALU.subtract, [r_S[1][t_], r_S[2][t_]], [r_S[1][t_]])
        for t_ in range(NTT):
            act(S_[1][t_], S_[1][t_], AF.Sqrt, [r_S[1][t_]], [r_S[1][t_]])
        for t_ in range(NTT):
            P.op(DVE, lambda e, o=S_[1][t_]: e.reciprocal(out=o, in_=o), reads=[r_S[1][t_]], writes=[r_S[1][t_]])
        n = 0
        for t_ in range(NTT):
            cond = 0 if t_ < 2 else 1
            mean, rstd = S_[0][t_], S_[1][t_]
            for k in range(8):
                q = n % 4
                n += 1
                tt(DVE, tmp[q], x[:, k, tsl(t_)], mean, ALU.subtract, [r_x[k][t_], r_S[0][t_]], [r_tmp[q]])
                tt(DVE, tmp[q], tmp[q], rstd, ALU.mult, [r_tmp[q], r_S[1][t_]], [r_tmp[q]])
                act(x[:, k, tsl(t_)], tmp[q], AF.Identity, [r_tmp[q], r_c], [r_x[k][t_]], scale=lnp[:, l, w, 0, k:k + 1], bias=lnp[:, l, w, 1, k:k + 1])
                if then_hb:
                    ts(POOL, hb[:, k, tsl(t_)], x[:, k, tsl(t_)], mod_ap(l, 4, k, cond), mod_ap(l, 3, k, cond), ALU.mult, ALU.add,
                       [r_x[k][t_], r_modl[l]], [r_hb[k][t_]])

    def retention(l, jl):
        phase_switch()
        qT = av(0, BF16, [128, 2, 1024]); r_qT = [[Res(True) for _ in range(2)] for _ in range(2)]
        kT = av(4096, BF16, [128, 2, 1024]); r_kT = [[Res(True) for _ in range(2)] for _ in range(2)]
        v = av(8192, BF16, [128, 8, 512]); r_v = [Res(True) for _ in range(8)]
        sg = av(16384, BF16, [128, 4, 1024]); r_sg = [[Res(True) for _ in range(2)] for _ in range(4)]
        S0 = av(24576, BF16, [128, 2, 2, 512]); r_S0 = [Res(True) for _ in range(2)]
        qdec = av(28672, BF16, [128, 2, 2, 512]); r_qdec = [Res(True) for _ in range(2)]
        kdec = av(24576, BF16, [128, 8, 2, 256]); r_kdec = r_S0 + r_qdec
        PT = [av(32768 + i * 1024, BF16, [128, 512]) for i in range(2)]; r_PT = [Res(True) for _ in range(2)]
        Ttab = av(34816, F32, [128, 1920]); r_T = Res(True)
        obf = av(42496, BF16, [128, 4, 512]); r_obf = Res(True)
        osq = av(46592, BF16, [128, 4, 512]); r_osq = Res(True)
        ttmp = av(42496, F32, [128, 1920])
        stt_ = [av(50688 + i * 2048, F32, [128, 512]) for i in range(3)]; r_st = [Res(True) for _ in range(3)]
        rowdec = av(56832, F32, [128, 512]); r_rowdec = Res(True)
        qraw = [av(58880 + i * 1024, BF16, [128, 512]) for i in range(2)]; r_qraw = [Res(True) for _ in range(2)]
        stage = [av(56832, F32, [128, 512]), av(58880, F32, [128, 512])]
        r_stage = [[r_rowdec], r_qraw]
        t_mean, t_rstd, t_1 = stt_
        pcnt = [0]

        def pbank():
            pcnt[0] += 1
            return 6 + pcnt[0] % 2

        for hd in range(4):
            li = jl * 8
            lgf = lg[:, li + hd:li + hd + 1]
            lgb = lg[:, li + 4 + hd:li + 4 + hd + 1]
            nlgb = nlg[:, li + 4 + hd:li + 4 + hd + 1]
            wdma(1, retin_d[jl][:, 2048 + hd * 512:2048 + (hd + 1) * 512])
            wdma(0, retin_d[jl][:, hd * 256:(hd + 1) * 256], half=0)
            wdma(0, retin_d[jl][:, 1024 + hd * 256:1024 + (hd + 1) * 256], half=1)
            wdma(2, retin_d[jl][:, 4096 + hd * 512:4096 + (hd + 1) * 512])
            dma(POOL, ws[3].rearrange("p k f -> p (k f)").rearrange("p (k f) -> p k f", k=4),
                retout_d[jl][hd * 512:(hd + 1) * 512, :].rearrange("(k p) f -> p k f", p=128), [], [r_wsA[3], r_wsB[3]], "ws3")
            wo = ws[3].rearrange("p k f -> p (k f)").rearrange("p (k f) -> p k f", k=4)
            ada_bg_step(pbank)
            dma(SP, Ttab, delta_d, [], [r_T], "dlt0")
            dma(SP, ttmp, delta_d, [], [r_obf, r_osq], "dlt1")
            table_steps = [
                lambda: ts(DVE, Ttab, Ttab, 0.0, None, ALU.max, None, [r_T], [r_T]),
                lambda: act(Ttab, Ttab, AF.Exp, [r_T, r_lg], [r_T], scale=lgf),
                lambda: stt(DVE, Ttab, ttmp, 0.0, Ttab, ALU.is_equal, ALU.add, [r_obf, r_osq, r_T], [r_T]),
                lambda: ts(DVE, ttmp, ttmp, 0.0, None, ALU.min, None, [r_obf, r_osq], [r_obf, r_osq]),
                lambda: act(ttmp, ttmp, AF.Exp, [r_obf, r_osq, r_lg], [r_obf, r_osq], scale=nlgb),
                lambda: tt(DVE, Ttab, Ttab, ttmp, ALU.mult, [r_T, r_obf, r_osq], [r_T]),
                lambda: act(decs[:, 0:2], jexp[:, 0:2], AF.Exp, [r_c, r_lg], [r_decs], scale=lgf),
                lambda: act(decs[:, 2:4], jexp[:, 2:4], AF.Exp, [r_c, r_lg], [r_decs], scale=lgb),
                lambda: ts(DVE, decs, decs, 0.0625, None, ALU.mult, None, [r_decs], [r_decs]),
            ]
            for g in range(2):
                base = g * 1024
                cond = g
                isA = (g == 0)
                if isA:
                    for dr in range(2):
                        dma(POOL, S0[:, dr], state_d[jl, dr, hd].rearrange("(c p) e -> p c e", p=128), [], [r_S0[dr]], "S0%d" % dr)
                for tc in range(8):
                    b = pbank()
                    t_ = g * 2 + tc // 4
                    for k in range(8):
                        mm(ps[b][:], hb[:, k, base + tc * 128:base + (tc + 1) * 128], ws[1][:, k, :], k == 0, k == 7,
                           [r_wsA[1], r_wsB[1], r_hb[k][t_]], rps[b])
                    if tc % 2 == 0:
                        act(v[:, tc, :], ps[b][:], AF.Copy, [rps[b]], [r_v[tc]])
                    else:
                        cp(DVE, v[:, tc, :], ps[b][:], [rps[b]], [r_v[tc]])
                    if isA and table_steps:
                        table_steps.pop(0)()
                for c4 in range(4):
                    dest, rdest = (qT, r_qT) if c4 < 2 else (kT, r_kT)
                    c = c4 % 2
                    scl = 1.0 if c4 < 2 else 0.0625
                    rw = r_wsA[0] if c4 < 2 else r_wsB[0]
                    for t2 in range(2):
                        b = pbank()
                        t_ = g * 2 + t2
                        for k in range(8):
                            mm(ps[b][:], ws[0][:, k, c4 * 128:(c4 + 1) * 128], hb[:, k, tsl(t_)], k == 0, k == 7, [rw, r_hb[k][t_]], rps[b])
                        d_ap = dest[:, c, t2 * 512:(t2 + 1) * 512]
                        if not isA:
                            act(d_ap, ps[b][:], AF.Identity, [rps[b]], [rdest[c][t2]], scale=scl)
                        else:
                            qq = pcnt[0] % 2
                            act(qraw[qq], ps[b][:], AF.Identity, [rps[b]], [r_qraw[qq]], scale=scl)
                            b2 = pbank()
                            mm(ps[b2][:], rotb, qraw[qq], True, True, [r_c, r_qraw[qq]], rps[b2])
                            if c == 0:
                                cs = rope[:, 128 + t2 * 8:128 + (t2 + 1) * 8].unsqueeze(2).to_broadcast([128, 8, 64])
                                sn = rope[:, 144 + t2 * 8:144 + (t2 + 1) * 8].unsqueeze(2).to_broadcast([128, 8, 64])
                            else:
                                cs = rope[:, 0:64].unsqueeze(1).to_broadcast([128, 8, 64])
                                sn = rope[:, 64:128].unsqueeze(1).to_broadcast([128, 8, 64])
                            r3 = "p (r c) -> p r c"
                            tt(DVE, t_mean.rearrange(r3, r=8), qraw[qq].rearrange(r3, r=8), cs, ALU.mult, [r_qraw[qq], r_c], [r_st[0]])
                            tt(DVE, t_rstd.rearrange(r3, r=8), ps[b2][:].rearrange(r3, r=8), sn, ALU.mult, [rps[b2], r_c], [r_st[1]])
                            tt(DVE, d_ap, t_mean, t_rstd, ALU.add, [r_st[0], r_st[1]], [rdest[c][t2]])
                while isA and table_steps:
                    table_steps.pop(0)()
                ada_bg_step(pbank)
                def deferred_proj():
                    for c in range(4):
                        for t2 in range(2):
                            b = pbank()
                            t_ = g * 2 + t2
                            for k in range(8):
                                mm(ps[b][:], ws[2][:, k, c * 128:(c + 1) * 128], hb[:, k, tsl(t_)], k == 0, k == 7,
                                   [r_wsA[2], r_wsB[2], r_hb[k][t_]], rps[b])
                            act(sg[:, c, t2 * 512:(t2 + 1) * 512], ps[b][:], AF.Silu, [rps[b]], [r_sg[c][t2]])
                    if not isA:
                        for tc in range(8):
                            b = pbank()
                            t_ = g * 2 + tc // 4
                            for k in range(8):
                                mm(ps[b][:, 0:256], hb[:, k, base + tc * 128:base + (tc + 1) * 128], ws[0][:, k, 256:512], k == 0, k == 7,
                                   [r_wsB[0], r_hb[k][t_]], rps[b])
                            hf = tc % 2
                            ts(DVE, kdec[:, tc, 0, :], ps[b][:, 0:256], decs[:, hf:hf + 1], None, ALU.mult, None, [rps[b], r_decs], r_kdec)
                            ts(DVE, kdec[:, tc, 1, :], ps[b][:, 0:256], decs[:, 2 + hf:3 + hf], None, ALU.mult, None, [rps[b], r_decs], r_kdec)
                        sc_ = 0
                        for s in range(4):
                            for dr in range(2):
                                for dc in range(2):
                                    b = pbank()
                                    for hf in range(2):
                                        tc = 2 * s + hf
                                        mm(ps[b][:], kdec[:, tc, dr, dc * 128:(dc + 1) * 128], v[:, tc, :], hf == 0, hf == 1,
                                           r_kdec + [r_v[tc]], rps[b])
                                    q = sc_ % 2
                                    sc_ += 1
                                    if q == 0:
                                        act(stage[q], ps[b][:], AF.Copy, [rps[b]], r_stage[q])
                                    else:
                                        cp(DVE, stage[q], ps[b][:], [rps[b]], r_stage[q])
                                    st_outs.append(dma(SP, st_d[s, jl, dr, hd, dc * 128:(dc + 1) * 128, :], stage[q], r_stage[q], [], "stg%d" % q))

                    ada_bg_step(pbank)

                dstate = [True]
                seqs = [(0, 1024, 512)] if isA else [(s * 256, 256, 256) for s in range(4)]
                for (sb0, N, NI) in seqs:
                    NJ = N // 128
                    for it in range(N // NI):
                        i0 = it * NI
                        l0 = sb0 + i0
                        t2 = l0 // 512
                        if isA:
                            for dr in range(2):
                                if dr == 0:
                                    act(rowdec, ip1[:, i0:i0 + NI], AF.Exp, [r_c, r_lg], [r_rowdec], scale=lgf)
                                else:
                                    act(rowdec, ip1[:, i0:i0 + NI], AF.Exp, [r_c, r_lg], [r_rowdec], scale=nlgb,
                                        bias=b1025[:, li + 4 + hd:li + 4 + hd + 1])
                                for dc in range(2):
                                    tt(DVE, qdec[:, dr, dc, :], qT[:, dc, l0:l0 + NI], rowdec, ALU.mult, [r_qT[dc][t2], r_rowdec], [r_qdec[dr]])
                        for jc in range(NJ):
                            b = jc % 2
                            j0 = sb0 + jc * 128
                            for dc in range(2):
                                mm(ps[b][:, 0:NI], kT[:, dc, j0:j0 + 128], qT[:, dc, l0:l0 + NI], dc == 0, dc == 1,
                                   [r_kT[dc][j0 // 512], r_qT[dc][t2]], rps[b])
                            off = i0 - 128 * jc + 896
                            tt(DVE, PT[b][:, 0:NI], ps[b][:, 0:NI], Ttab[:, off:off + NI], ALU.mult, [rps[b], r_T], [r_PT[b]])
                            tcj = j0 // 128
                            for ec in range(4):
                                mm(ps[2 + ec][:, 0:NI], v[:, tcj, ec * 128:(ec + 1) * 128], PT[b][:, 0:NI], jc == 0, (jc == NJ - 1) and not isA,
                                   [r_v[tcj], r_PT[b]], rps[2 + ec])
                        if isA:
                            for dr in range(2):
                                for dc in range(2):
                                    for ec in range(4):
                                        mm(ps[2 + ec][:, 0:NI], S0[:, dr, dc, ec * 128:(ec + 1) * 128], qdec[:, dr, dc, :], False,
                                           dr == 1 and dc == 1, [r_S0[dr], r_qdec[dr]], rps[2 + ec])
                        if dstate[0]:
                            dstate[0] = False
                            deferred_proj()
                        for ec in range(4):
                            act(obf[:, ec, 0:NI], ps[2 + ec][:, 0:NI], AF.Copy, [rps[2 + ec]], [r_obf])
                            act(osq[:, ec, 0:NI], ps[2 + ec][:, 0:NI], AF.Square, [rps[2 + ec]], [r_osq])
                        for ec in range(4):
                            mm(ps[0][:, 0:NI], onesb, obf[:, ec, 0:NI], ec == 0, ec == 3, [r_c, r_obf], rps[0])
                        for ec in range(4):
                            mm(ps[1][:, 0:NI], onesb, osq[:, ec, 0:NI], ec == 0, ec == 3, [r_c, r_osq], rps[1])
                        m_, r_, t1 = t_mean[:, 0:NI], t_rstd[:, 0:NI], t_1[:, 0:NI]
                        ts(DVE, m_, ps[0][:, 0:NI], 1.0 / 512, None, ALU.mult, None, [rps[0]], [r_st[0]])
                        tt(DVE, t1, m_, m_, ALU.mult, [r_st[0]], [r_st[2]])
                        ts(DVE, r_, ps[1][:, 0:NI], 1.0 / 512, LN_EPS, ALU.mult, ALU.add, [rps[1]], [r_st[1]])
                        tt(DVE, r_, r_, t1, ALU.subtract, [r_st[1], r_st[2]], [r_st[1]])
                        act(r_, r_, AF.Sqrt, [r_st[1]], [r_st[1]])
                        P.op(DVE, lambda e, o=r_, i=r_: e.reciprocal(out=o, in_=i), reads=[r_st[1]], writes=[r_st[1]])
                        for ec in range(4):
                            tt(DVE, t1, ps[2 + ec][:, 0:NI], m_, ALU.subtract, [rps[2 + ec], r_st[0]], [r_st[2]])
                            tt(DVE, t1, t1, r_, ALU.mult, [r_st[2], r_st[1]], [r_st[2]])
                            sga = sg[:, ec, l0:l0 + NI]
                            tt(DVE, sga, t1, sga, ALU.mult, [r_st[2], r_sg[ec][t2]], [r_sg[ec][t2]])
                        for dc in range(8):
                            b = pbank()
                            for ec in range(4):
                                mm(ps[b][:, 0:NI], wo[:, ec, dc * 128:(dc + 1) * 128], sg[:, ec, l0:l0 + NI], ec == 0, ec == 3,
                                   [r_wsA[3], r_wsB[3], r_sg[ec][t2]], rps[b])
                            gt = base + l0
                            xs = x[:, dc, gt:gt + NI]
                            stt(DVE, xs, ps[b][:, 0:NI], mod_ap(l, 2, dc, cond), xs, ALU.mult, ALU.add,
                                [rps[b], r_modl[l], r_x[dc][gt // 512]], [r_x[dc][gt // 512]])

        ada_bg_finish(pbank)

    def conv(l, jl):
        phase_switch()
        cb = [0]

        def cbank():
            cb[0] += 1
            return 6 + cb[0] % 2
        zT = av(0, BF16, [128, 8, T]); r_z = [[Res(True) for _ in range(NTT)] for _ in range(8)]
        cgs = [av(32768 + i * 2048, F32, [128, 512]) for i in range(2)]; r_cgs = [Res(True) for _ in range(2)]
        u = [av(36864 + i * 2048, F32, [128, 512]) for i in range(2)]; r_u = [Res(True) for _ in range(2)]
        cu = [av(40960 + i * 2048, F32, [128, 512]) for i in range(2)]; r_cu = [Res(True) for _ in range(2)]
        n = 0
        for half in range(2):
            for i in range(3):
                wdma(i, cvin_d[jl][:, i * 1024 + half * 512:i * 1024 + (half + 1) * 512])
            if half == 0:
                wdma(3, cvout_d[jl][:, 0:512])
            for c in range(4):
                cc = half * 4 + c
                ada_bg_step(cbank)
                for t_ in range(NTT):
                    q = n % 2
                    n += 1
                    bb = [q, 2 + q, 4 + q]
                    for i in range(3):
                        for k in range(8):
                            mm(ps[bb[i]][:], ws[i][:, k, c * 128:(c + 1) * 128], hb[:, k, tsl(t_)], k == 0, k == 7,
                               [r_wsA[i], r_wsB[i], r_hb[k][t_]], rps[bb[i]])
                    act(cgs[q], ps[bb[1]][:], AF.Copy, [rps[bb[1]]], [r_cgs[q]])
                    tt(DVE, u[q], cgs[q], ps[bb[2]][:], ALU.mult, [r_cgs[q], rps[bb[2]]], [r_u[q]])
                    act(cu[q], u[q], AF.Identity, [r_u[q], r_c], [r_cu[q]], scale=cvw[:, jl, 1, cc:cc + 1])
                    R_ = 8 if t_ < 2 else 2
                    u3 = u[q].rearrange("p (r c) -> p r c", r=R_)
                    c3 = cu[q].rearrange("p (r c) -> p r c", r=R_)
                    stt(DVE, c3[:, :, 1:], u3[:, :, :-1], cvw[:, jl, 0, cc:cc + 1], c3[:, :, 1:], ALU.mult, ALU.add, [r_u[q], r_cu[q], r_c], [r_cu[q]])
                    stt(DVE, c3[:, :, :-1], u3[:, :, 1:], cvw[:, jl, 2, cc:cc + 1], c3[:, :, :-1], ALU.mult, ALU.add, [r_u[q], r_cu[q], r_c], [r_cu[q]])
                    tt(DVE, zT[:, cc, tsl(t_)], ps[bb[0]][:], cu[q], ALU.mult, [rps[bb[0]], r_cu[q]], [r_z[cc][t_]])
        wdma(0, cvout_d[jl][:, 512:1024])
        n = 0
        for dcb in range(2):
            s = 3 if dcb == 0 else 0
            for j in range(4):
                dc = dcb * 4 + j
                ada_bg_step(cbank)
                for t_ in range(NTT):
                    b = 6 + n % 2
                    n += 1
                    for k in range(8):
                        mm(ps[b][:], ws[s][:, k, j * 128:(j + 1) * 128], zT[:, k, tsl(t_)], k == 0, k == 7,
                           [r_wsA[s], r_wsB[s], r_z[k][t_]], rps[b])
                    cond = 0 if t_ < 2 else 1
                    xs = x[:, dc, tsl(t_)]
                    stt(DVE, xs, ps[b][:], mod_ap(l, 2, dc, cond), xs, ALU.mult, ALU.add, [rps[b], r_modl[l], r_x[dc][t_]], [r_x[dc][t_]])

        ada_bg_finish(cbank)

    def moe(l):
        phase_switch()
        hidb = [av(0, BF16, [128, 4, T]), av(32768, BF16, [128, 4, T])]
        r_hidb = [[[Res(True) for _ in range(NTT)] for _ in range(4)] for _ in range(2)]
        sgt = [av(16384 + i * 2048, F32, [128, 512]) for i in range(2)]; r_sgt = [Res(True) for _ in range(2)]
        tm = [av(20480 + i * 2048, F32, [128, 512]) for i in range(2)]; r_tm = [Res(True) for _ in range(2)]
        cHi = av(24576, BF16, [128, T]); cLo = av(28672, BF16, [128, T]); r_combT = [Res(True) for _ in range(NTT)]
        o = 32768
        SZ = 16 * E * 4
        scores = av(o, F32, [128, 16, E]); r_sc = Res(True)
        sel = av(o + SZ, F32, [128, 16, E]); r_sel = Res(True)
        tA = av(o + 2 * SZ, F32, [128, 16, E]); r_tA = Res(True)
        comb = av(o + 3 * SZ, F32, [128, 16, E]); r_comb = Res(True)
        o2 = o + 4 * SZ
        m1 = av(o2, F32, [128, 128]); m2 = av(o2 + 512, F32, [128, 128]); grp = av(o2 + 1024, F32, [128, 128])
        top8 = av(o2 + 1536, F32, [128, 16, 8]); gm = av(o2 + 2048, F32, [128, 128]); top8e = av(o2 + 2560, F32, [128, 16, 8])
        den = av(o2 + 3072, F32, [128, 16])
        r_sm = Res(True)

        dma(POOL, wr, rout_d[l].rearrange("(k p) f -> p k f", p=128), [], [r_wr], "wr")
        for tc in range(16):
            b = 6 + tc // 8
            t_ = tc // 4
            for k in range(8):
                mm(ps[b][:, (tc % 8) * E:(tc % 8 + 1) * E], hb[:, k, tc * 128:(tc + 1) * 128], wr[:, k, :], k == 0, k == 7,
                   [r_wr, r_hb[k][t_]], rps[b], acc=not (tc % 8 == 0 and k == 0))
        for b_ in range(2):
            act(scores[:, b_ * 8:(b_ + 1) * 8, :].rearrange("p a e -> p (a e)"), ps[6 + b_][:, 0:8 * E], AF.Sigmoid, [rps[6 + b_]], [r_sc])
        g3 = "p a (g e) -> p (a g) e"
        tt(DVE, sel, scores, mbias[:, l, :].unsqueeze(1).to_broadcast([128, 16, E]), ALU.add, [r_sc, r_c], [r_sel])
        selg = sel.rearrange(g3, g=8)
        tAg = tA.rearrange(g3, g=8)
        P.op(DVE, lambda e: e.tensor_reduce(out=m1, in_=selg, axis=AX.X, op=ALU.max), reads=[r_sel], writes=[r_sm])
        tt(DVE, tAg, selg, m1.unsqueeze(2).to_broadcast([128, 128, PG]), ALU.is_equal, [r_sel, r_sm], [r_tA])
        stt(DVE, tA, tA, -1e9, sel, ALU.mult, ALU.add, [r_tA, r_sel], [r_tA])
        P.op(DVE, lambda e: e.tensor_reduce(out=m2, in_=tAg, axis=AX.X, op=ALU.max), reads=[r_tA], writes=[r_sm])
        tt(DVE, grp, m1, m2, ALU.add, [r_sm], [r_sm])
        for tc in range(16):
            P.op(DVE, lambda e, tc=tc: e.max(out=top8[:, tc, :], in_=grp[:, tc * 8:(tc + 1) * 8]), reads=[r_sm], writes=[r_sm])
        tt(DVE, gm.rearrange("p (a g) -> p a g", a=16), grp.rearrange("p (a g) -> p a g", a=16),
           top8[:, :, 3:4].to_broadcast([128, 16, 8]), ALU.is_ge, [r_sm], [r_sm])
        ts(DVE, tA, sel, 2.0, None, ALU.add, None, [r_sel], [r_tA])
        tt(DVE, tAg, tAg, gm.unsqueeze(2).to_broadcast([128, 128, PG]), ALU.mult, [r_tA, r_sm], [r_tA])
        for tc in range(16):
            P.op(DVE, lambda e, tc=tc: e.max(out=top8e[:, tc, :], in_=tA[:, tc, :]), reads=[r_tA], writes=[r_sm])
        tt(DVE, sel, tA, top8e[:, :, 5:6].to_broadcast([128, 16, E]), ALU.is_ge, [r_tA, r_sm], [r_sel])
        tt(DVE, tA, scores, sel, ALU.mult, [r_sc, r_sel], [r_tA])
        P.op(DVE, lambda e: e.tensor_reduce(out=den, in_=tA, axis=AX.X, op=ALU.add), reads=[r_tA], writes=[r_sm])
        P.op(DVE, lambda e: e.reciprocal(out=den, in_=den), reads=[r_sm], writes=[r_sm])
        ts(DVE, den, den, 2.5, None, ALU.mult, None, [r_sm], [r_sm])
        tt(DVE, comb, tA, den.unsqueeze(2).to_broadcast([128, 16, E]), ALU.mult, [r_tA, r_sm], [r_comb])
        for tc in range(16):
            b = 6 + (tc // 4) % 2
            tr(ps[b][0:E, (tc % 4) * 128:(tc % 4 + 1) * 128], comb[:, tc, :], [r_comb], rps[b])
            if tc % 4 == 3:
                t_ = tc // 4
                act(cHi[0:E, tsl(t_)], ps[b][0:E, :], AF.Copy, [rps[b]], [r_combT[t_]])
                tt(DVE, cLo[0:E, tsl(t_)], ps[b][0:E, :], cHi[0:E, tsl(t_)], ALU.subtract, [rps[b], r_combT[t_]], [r_combT[t_]])

        NX = E + 1
        wdA = [av(52288, BF16, [128, 2, 1024]), av(52288 + 4096, BF16, [128, 2, 1024])]
        wd4 = [wdS[0], wdS[1], wdA[0], wdA[1]]
        r_wd4 = [r_wd[0], r_wd[1], Res(True), Res(True)]
        router_res = [r_sc, r_sel, r_tA, r_comb, r_sm]
        pairs = [[e for e in (2 * p, 2 * p + 1) if e < NX] for p in range((NX + 1) // 2)]

        def load_gu(e):
            s = e % 4
            if e < E:
                wdma(s, wg_d[l, e], half=0)
                wdma(s, wu_d[l, e], half=1)
            else:
                wdma(s, sg_d[l], half=0)
                wdma(s, su_d[l], half=1)

        def load_d(p):
            for e in pairs[p]:
                s = (p % 2) * 2 + e % 2
                src = wd_d[l, e] if e < E else sd_d[l]
                dma(POOL, wd4[s], src.rearrange("(k p) f -> p k f", p=128), [], [r_wd4[s]], "wd%d" % s)

        cnt = [0]
        dn = [0]
        first_b = [True] * 16

        def compute_block(p, e, t_, drain_counts=(0, 0)):
            s = e % 4
            m = e % 2
            hid, r_hid = hidb[p % 2], r_hidb[p % 2]
            bcb = 4 if t_ % 2 == 0 else 6
            if e < E:
                sel1 = identb[0:E, e:e + 1].to_broadcast([E, 128])
                mm(ps[bcb][:], sel1, cHi[0:E, tsl(t_)], True, False, [r_c, r_combT[t_]], rps[bcb])
                mm(ps[bcb][:], sel1, cLo[0:E, tsl(t_)], False, True, [r_c, r_combT[t_]], rps[bcb])
            for fc in range(2):
                q = cnt[0] % 2
                cnt[0] += 1
                for k in range(8):
                    mm(ps[q][:], ws[s][:, k, fc * 128:(fc + 1) * 128], hb[:, k, tsl(t_)], k == 0, k == 7, [r_wsA[s], r_hb[k][t_]], rps[q])
                for k in range(8):
                    mm(ps[2 + q][:], ws[s][:, k, 256 + fc * 128:256 + (fc + 1) * 128], hb[:, k, tsl(t_)], k == 0, k == 7,
                       [r_wsB[s], r_hb[k][t_]], rps[2 + q])
                act(sgt[q], ps[q][:], AF.Silu, [rps[q]], [r_sgt[q]])
                hd_ = hid[:, m * 2 + fc, tsl(t_)]
                wres = [r_hid[m * 2 + fc][t_]]
                if p % 2 == 1 and first_b[(m * 2 + fc) * 4 + t_]:
                    first_b[(m * 2 + fc) * 4 + t_] = False
                    wres = wres + router_res
                if e < E:
                    tt(DVE, tm[q], sgt[q], ps[2 + q][:], ALU.mult, [r_sgt[q], rps[2 + q]], [r_tm[q]])
                    tt(DVE, hd_, tm[q], ps[bcb][:], ALU.mult, [r_tm[q], rps[bcb]], wres)
                else:
                    tt(DVE, hd_, sgt[q], ps[2 + q][:], ALU.mult, [r_sgt[q], rps[2 + q]], wres)
                for _ in range(drain_counts[fc]):
                    if pending:
                        pp, tq, dq = pending.pop(0)
                        down_group(pp, tq, dq)

        def down_group(p, t_, dc):
            members = pairs[p]
            hid, r_hid = hidb[p % 2], r_hidb[p % 2]
            cond = 0 if t_ < 2 else 1
            b = 5 + dn[0] % 2 * 2
            dn[0] += 1
            n_ = len(members) * 2
            i = 0
            for e in members:
                m = e % 2
                s = (p % 2) * 2 + m
                for fc in range(2):
                    mm(ps[b][:], wd4[s][:, fc, dc * 128:(dc + 1) * 128], hid[:, m * 2 + fc, tsl(t_)], i == 0, i == n_ - 1,
                       [r_wd4[s], r_hid[m * 2 + fc][t_]], rps[b])
                    i += 1
            xs = x[:, dc, tsl(t_)]
            stt(DVE, xs, ps[b][:], mod_ap(l, 5, dc, cond), xs, ALU.mult, ALU.add, [rps[b], r_modl[l], r_x[dc][t_]], [r_x[dc][t_]])

        PF = 2
        for e in range(min(PF, NX)):
            load_gu(e)
        load_d(0)
        if len(pairs) > 1:
            load_d(1)
        pending = []
        for p, members in enumerate(pairs):
            nslots = len(members) * NTT
            per = -(-len(pending) // nslots) if pending else 0
            for e in members:
                if e + PF < NX:
                    load_gu(e + PF)
                for t_ in range(NTT):
                    compute_block(p, e, t_, ((per + 1) // 2, per // 2))
            while pending:
                pp, tq, dq = pending.pop(0)
                down_group(pp, tq, dq)
            if p >= 1 and p + 1 < len(pairs):
                load_d(p + 1)
            pending = [(p, tq, dq) for tq in range(NTT) for dq in range(8)]
        while pending:
            pp, tq, dq = pending.pop(0)
            down_group(pp, tq, dq)

    st_outs = []
    make_hb(0, 1, 0, [DVE, POOL])
    for k in range(8):
        for t_ in range(NTT):
            ts(POOL, x[:, k, tsl(t_)], x[:, k, tsl(t_)], ALPHA, None, ALU.mult, None, [r_x[k][t_]], [r_x[k][t_]])
    done = False
    for l in range(L):
        jl = l // 2
        if l > 0:
            make_hb(l, 1, 0, [DVE, POOL])
        if l + 1 < L:
            ada_bg_start(l + 1)
        if l % 2 == 0:
            retention(l, jl)
        else:
            conv(l, jl)
        if stop_after == (l, "mix"):
            break
        layer_norm(l, 0, True)
        if stop_after == (l, "ln1"):
            break
        moe(l)
        if stop_after == (l, "moe"):
            break
        layer_norm(l, 1, False)
    outs = []
    for k in range(8):
        outs.append(dma(SP, yT_d[k * 128:(k + 1) * 128, :], x[:, k, :], r_x[k], [], "out%d" % k))
    P.emit(final_waits=outs + st_outs[-2:])
    return nc, P


def const_inputs():
    p = np.arange(128)
    ident = np.eye(128, dtype=np.float32)
    rot = np.zeros((128, 128), np.float32)
    for m in range(64):
        rot[m + 64, m] = -1.0
        rot[m, m + 64] = 1.0
    delta = (np.arange(1920)[None, :] - p[:, None] - 896).astype(np.float32)
    ip1 = np.broadcast_to(np.arange(1, 1025, dtype=np.float32)[None, :], (128, 1024)).copy()
    jexp = np.stack([255.0 - p, 255.0 - (128 + p), 0.0 + p, 128.0 + p], axis=1).astype(np.float32)
    freqs = (10000.0 ** (-np.arange(64, dtype=np.float32) / 64)).astype(np.float32)
    fr = freqs[p % 64]
    angC = np.arange(64, dtype=np.float32)[None, :] * fr[:, None]
    angR = np.arange(16, dtype=np.float32)[None, :] * fr[:, None]
    rope = np.concatenate([np.cos(angC), np.sin(angC), np.cos(angR), np.sin(angR)], axis=1).astype(np.float32)
    return {"ident": ident, "rotm": rot, "delta": delta, "ip1": ip1, "jexp": jexp, "rope": rope}


def per_core_inputs(inp, core, L, E):
    NR, NCV = (L + 1) // 2, L // 2
    f = np.float32
    xs = np.asarray(inp["x_sample"][core], f)
    xp = np.asarray(inp["x_prompt"][4 * core:4 * core + 4], f).reshape(1024, 1024)
    xT = np.ascontiguousarray(np.concatenate([xs, xp], axis=0).T)
    cond = np.stack([np.asarray(inp["c"][core], f), np.asarray(inp["c_ctx"], f)], axis=1)
    condT = np.ascontiguousarray(cond.reshape(8, 128, 2).transpose(1, 0, 2))
    d = {"xT": xT, "condT": condT, "state": np.ascontiguousarray(np.asarray(inp["state_ret"][core], f))}
    return d


def shared_inputs(inp, L, E):
    NR, NCV = (L + 1) // 2, L // 2
    f = np.float32
    d = {}
    for k in ("ada_w", "ret_w_in", "ret_w_out", "moe_router", "moe_w_gate", "moe_w_up", "moe_w_down",
              "shared_w_gate", "shared_w_up", "shared_w_down"):
        d[k] = np.ascontiguousarray(np.asarray(inp[k], f))
    if NCV:
        d["conv_w_in"] = np.ascontiguousarray(np.asarray(inp["conv_w_in"], f))
        d["conv_w_out"] = np.ascontiguousarray(np.asarray(inp["conv_w_out"], f))
        cw = np.asarray(inp["conv_w"], f)
        d["convw"] = np.ascontiguousarray(cw.reshape(NCV, 3, 8, 128).transpose(3, 0, 1, 2))
    ab = np.asarray(inp["ada_b"], f).reshape(L, 48, 128).transpose(2, 0, 1)
    d["adab"] = np.ascontiguousarray(np.repeat(ab[:, :, :, None], 2, axis=3))
    g = np.asarray(inp["ln_g"], f).reshape(L, 2, 8, 128)
    b = np.asarray(inp["ln_b"], f).reshape(L, 2, 8, 128)
    d["lnp"] = np.ascontiguousarray(np.stack([g, b], axis=2).transpose(4, 0, 1, 2, 3))
    d["rdecay"] = np.ascontiguousarray(np.broadcast_to(np.asarray(inp["ret_decay"], f).reshape(1, NR * 8), (128, NR * 8)))
    d["mbias"] = np.ascontiguousarray(np.broadcast_to(np.asarray(inp["moe_bias"], f)[None], (128, L, E)))
    d.update(const_inputs())
    return d


_CACHE = {}


def run(inp, n_cores, L=4, E=64, stop_after=None, trace=False):
    key = (L, E, stop_after)
    if key not in _CACHE:
        _CACHE[key] = build(L, E, stop_after)
    nc, P = _CACHE[key]
    sh = shared_inputs(inp, L, E)
    in_maps = []
    for c in range(n_cores):
        m = dict(sh)
        m.update(per_core_inputs(inp, c, L, E))
        in_maps.append(m)
    res = run_bass_kernel_spmd(nc, in_maps, core_ids=list(range(n_cores)), trace=trace)
    return res


def kernel(**inputs):
    res = run(inputs, 8)
    NR = 2
    y_s = np.empty((8, 1024, 1024), np.float32)
    y_p = np.empty((32, 256, 1024), np.float32)
    st = np.empty((32, NR, 2, 4, 256, 512), np.float32)
    for c in range(8):
        yT = np.asarray(res.results[c]["yT"])
        y = yT.T
        y_s[c] = y[0:1024]
        y_p[4 * c:4 * c + 4] = y[1024:2048].reshape(4, 256, 1024)
        st[4 * c:4 * c + 4] = np.asarray(res.results[c]["st"])
    return (y_p, y_s, st)
```
